# Optimizing a Trainium2 kernel written in Bass

```python
import jax, jax.numpy as jnp
from jax import lax
import numpy as np

D_MODEL = 1024
BATCH = 8
SEQ = 4096
DEPTH = 4

LRU_WIDTH = D_MODEL
LRU_HEADS = 8
LRU_HEAD_DIM = LRU_WIDTH // LRU_HEADS
CONV_WIDTH = 4
LRU_C = 8.0
POOL_WINDOWS = (2, 4, 8, 16)
POOL_GROUPS = len(POOL_WINDOWS)
POOL_WIDTH = D_MODEL // 2
POOL_GROUP_DIM = POOL_WIDTH // POOL_GROUPS
N_BRANCHES = 2
IN_PROJ_WIDTH = 2 * LRU_WIDTH + POOL_WIDTH + N_BRANCHES * D_MODEL
N_EXPERTS = 32
TOP_K = 4
D_FF_EXPERT = D_MODEL
SWIGLU_LIMIT = 7.0
SWIGLU_ALPHA = 1.702
EXPERT_BLOCK = 128
NORM_EPS = 1e-6

kernel_name = 'hybrid_rglru_pool_moe_adaln'


def _rmsnorm(x, g):
    xf = x.astype(jnp.float32)
    y = xf * lax.rsqrt(jnp.mean(xf * xf, axis=-1, keepdims=True) + NORM_EPS)
    return (y * g.astype(jnp.float32)).astype(x.dtype)


def _modulate(h, shift, scale):
    return h * (1 + scale[:, None, :]) + shift[:, None, :]


def _block_diag(x, w):
    bsz, s, _ = x.shape
    n, di, do = w.shape
    y = jnp.einsum('bsgi,gio->bsgo', x.reshape(bsz, s, n, di), w)
    return y.reshape(bsz, s, n * do)


def _causal_depthwise_conv(x, w, b):
    y = lax.conv_general_dilated(x, w[:, None, :], window_strides=(1,), padding=[(CONV_WIDTH - 1, 0)], dimension_numbers=('NWC', 'WIO', 'NWC'), feature_group_count=x.shape[-1])
    return y + b


def _rg_lru(x, w_a, b_a, w_x, b_x, lam):
    r = jax.nn.sigmoid((_block_diag(x, w_a) + b_a).astype(jnp.float32))
    i = jax.nn.sigmoid((_block_diag(x, w_x) + b_x).astype(jnp.float32))
    log_a = -LRU_C * r * jax.nn.softplus(-lam.astype(jnp.float32))
    a = jnp.exp(log_a)
    mult = jnp.sqrt(-jnp.expm1(2.0 * log_a))
    u = mult * (i * x.astype(jnp.float32))

    def combine(left, right):
        a_l, b_l = left
        a_r, b_r = right
        return a_l * a_r, a_r * b_l + b_r

    _, h = lax.associative_scan(combine, (a, u), axis=1)
    return h.astype(x.dtype)


def _multiscale_pool(x):
    bsz, s, _ = x.shape
    xf = x.astype(jnp.float32)
    cs = lax.cumsum(xf, axis=1)
    w_max = max(POOL_WINDOWS)
    cs_pad = jnp.pad(cs, ((0, 0), (w_max, 0), (0, 0)))
    t = jnp.arange(s, dtype=jnp.float32)
    outs = []
    for g, w in enumerate(POOL_WINDOWS):
        lo, hi = g * POOL_GROUP_DIM, (g + 1) * POOL_GROUP_DIM
        win_sum = cs[:, :, lo:hi] - cs_pad[:, w_max - w:w_max - w + s, lo:hi]
        count = jnp.minimum(t + 1.0, float(w))[None, :, None]
        outs.append(win_sum / count - xf[:, :, lo:hi])
    return jnp.concatenate(outs, axis=-1).astype(x.dtype)


def _hybrid_mixer(h, w_in, conv_w, conv_b, w_rg_a, b_rg_a, w_rg_x, b_rg_x, lam, w_pool, pool_scale, w_up_a, w_up_b, w_out):
    bsz, s, _ = h.shape
    z = h @ w_in
    o1 = LRU_WIDTH
    o2 = o1 + LRU_WIDTH
    o3 = o2 + POOL_WIDTH
    x_lru, g_lru, x_pool, gate_logits = z[..., :o1], z[..., o1:o2], z[..., o2:o3], z[..., o3:]
    y_lru = _rg_lru(_causal_depthwise_conv(x_lru, conv_w, conv_b), w_rg_a, b_rg_a, w_rg_x, b_rg_x, lam)
    y_lru = y_lru * jax.nn.gelu(g_lru)
    y_pool = _block_diag(_multiscale_pool(x_pool), w_pool) * pool_scale
    p_a = y_lru @ w_up_a
    p_b = y_pool @ w_up_b
    gates = jax.nn.sigmoid(gate_logits).reshape(bsz, s, N_BRANCHES, D_MODEL)
    merged = gates[:, :, 0, :] * p_a + gates[:, :, 1, :] * p_b
    return merged @ w_out


def _moe(h, w_router, b_router, w_e_in, b_e_in, w_e_out, b_e_out):
    bsz, s, d = h.shape
    n_tok = bsz * s
    ht = h.reshape(n_tok, d)
    logits = (ht @ w_router + b_router).astype(jnp.float32)
    top_vals, top_idx = lax.top_k(logits, TOP_K)
    weights = jax.nn.softmax(top_vals, axis=-1)
    n_assign = n_tok * TOP_K
    flat_e = top_idx.reshape(n_assign).astype(jnp.int32)
    flat_tok = jnp.repeat(jnp.arange(n_tok, dtype=jnp.int32), TOP_K)
    flat_w = weights.reshape(n_assign)
    order = jnp.argsort(flat_e)
    sorted_e = flat_e[order]
    sorted_tok = flat_tok[order]
    sorted_w = flat_w[order]
    counts = jnp.bincount(flat_e, length=N_EXPERTS)
    padded = ((counts + EXPERT_BLOCK - 1) // EXPERT_BLOCK) * EXPERT_BLOCK
    start_sorted = jnp.cumsum(counts) - counts
    pad_end = jnp.cumsum(padded)
    start_pad = pad_end - padded
    dest = start_pad[sorted_e] + jnp.arange(n_assign, dtype=jnp.int32) - start_sorted[sorted_e]
    n_blocks = -(-n_assign // EXPERT_BLOCK) + N_EXPERTS
    n_slots = n_blocks * EXPERT_BLOCK
    buf_tok = jnp.zeros((n_slots,), jnp.int32).at[dest].set(sorted_tok)
    buf_w = jnp.zeros((n_slots,), jnp.float32).at[dest].set(sorted_w)
    block_start = jnp.arange(n_blocks, dtype=jnp.int32) * EXPERT_BLOCK
    block_e = jnp.minimum(jnp.searchsorted(pad_end, block_start, side='right'), N_EXPERTS - 1).astype(jnp.int32)

    def expert_block(args):
        tok, e = args
        xb = ht[tok]
        gu = xb @ w_e_in[e] + b_e_in[e]
        gate, up = gu[:, :D_FF_EXPERT], gu[:, D_FF_EXPERT:]
        gate = jnp.minimum(gate, SWIGLU_LIMIT)
        up = jnp.clip(up, -SWIGLU_LIMIT, SWIGLU_LIMIT)
        act = gate * jax.nn.sigmoid(SWIGLU_ALPHA * gate) * (up + 1)
        return act @ w_e_out[e] + b_e_out[e]

    outs = lax.map(expert_block, (buf_tok.reshape(n_blocks, EXPERT_BLOCK), block_e))
    y = jnp.zeros((n_tok, d), jnp.float32).at[buf_tok].add(outs.reshape(n_slots, d).astype(jnp.float32) * buf_w[:, None])
    return y.reshape(bsz, s, d).astype(h.dtype)


def setup_inputs(seed: int = 0) -> dict:
    key = jax.random.key(seed)
    ks = jax.random.split(key, 26)
    f32 = jnp.float32
    L, D = DEPTH, D_MODEL

    def nrm(k, shape, scale):
        return jax.random.normal(k, shape, f32) * scale

    u = jax.random.uniform(ks[14], (L, LRU_WIDTH), f32, 0.9, 0.999)
    s = u ** (1.0 / LRU_C)
    lru_lambda = jnp.log(s) - jnp.log1p(-s)
    return {
        'x': nrm(ks[0], (BATCH, SEQ, D), 1.0),
        'c': nrm(ks[1], (BATCH, D), 1.0),
        'norm1_g': 1.0 + nrm(ks[2], (L, D), 0.05),
        'norm2_g': 1.0 + nrm(ks[3], (L, D), 0.05),
        'w_ada': nrm(ks[4], (L, D, 6 * D), 0.5 * D ** -0.5),
        'b_ada': nrm(ks[5], (L, 6 * D), 0.02),
        'w_in': nrm(ks[6], (L, D, IN_PROJ_WIDTH), D ** -0.5),
        'conv_w': nrm(ks[7], (L, CONV_WIDTH, LRU_WIDTH), CONV_WIDTH ** -0.5),
        'conv_b': nrm(ks[8], (L, LRU_WIDTH), 0.02),
        'w_rg_a': nrm(ks[9], (L, LRU_HEADS, LRU_HEAD_DIM, LRU_HEAD_DIM), LRU_HEAD_DIM ** -0.5),
        'b_rg_a': nrm(ks[10], (L, LRU_WIDTH), 0.1),
        'w_rg_x': nrm(ks[11], (L, LRU_HEADS, LRU_HEAD_DIM, LRU_HEAD_DIM), LRU_HEAD_DIM ** -0.5),
        'b_rg_x': nrm(ks[12], (L, LRU_WIDTH), 0.1),
        'lru_lambda': lru_lambda,
        'w_pool': nrm(ks[13], (L, POOL_GROUPS, POOL_GROUP_DIM, POOL_GROUP_DIM), POOL_GROUP_DIM ** -0.5),
        'pool_scale': 1.0 + nrm(ks[15], (L, POOL_WIDTH), 0.1),
        'w_up_a': nrm(ks[16], (L, LRU_WIDTH, D), LRU_WIDTH ** -0.5),
        'w_up_b': nrm(ks[17], (L, POOL_WIDTH, D), POOL_WIDTH ** -0.5),
        'w_out': nrm(ks[18], (L, D, D), D ** -0.5),
        'w_router': nrm(ks[19], (L, D, N_EXPERTS), D ** -0.5),
        'b_router': nrm(ks[20], (L, N_EXPERTS), 0.01),
        'w_e_in': nrm(ks[21], (L, N_EXPERTS, D, 2 * D_FF_EXPERT), D ** -0.5),
        'b_e_in': nrm(ks[22], (L, N_EXPERTS, 2 * D_FF_EXPERT), 0.02),
        'w_e_out': nrm(ks[23], (L, N_EXPERTS, D_FF_EXPERT, D), D_FF_EXPERT ** -0.5),
        'b_e_out': nrm(ks[24], (L, N_EXPERTS, D), 0.02),
        'final_g': 1.0 + nrm(ks[25], (D,), 0.05),
    }


def reference(x, c, norm1_g, norm2_g, w_ada, b_ada, w_in, conv_w, conv_b, w_rg_a, b_rg_a, w_rg_x, b_rg_x, lru_lambda, w_pool, pool_scale, w_up_a, w_up_b, w_out, w_router, b_router, w_e_in, b_e_in, w_e_out, b_e_out, final_g):
    c_act = jax.nn.silu(c)
    for l in range(DEPTH):
        mod = c_act @ w_ada[l] + b_ada[l]
        shift1, scale1, gate1, shift2, scale2, gate2 = jnp.split(mod, 6, axis=-1)
        h = _modulate(_rmsnorm(x, norm1_g[l]), shift1, scale1)
        mix = _hybrid_mixer(h, w_in[l], conv_w[l], conv_b[l], w_rg_a[l], b_rg_a[l], w_rg_x[l], b_rg_x[l], lru_lambda[l], w_pool[l], pool_scale[l], w_up_a[l], w_up_b[l], w_out[l])
        x = x + gate1[:, None, :] * mix
        h = _modulate(_rmsnorm(x, norm2_g[l]), shift2, scale2)
        ffn = _moe(h, w_router[l], b_router[l], w_e_in[l], b_e_in[l], w_e_out[l], b_e_out[l])
        x = x + gate2[:, None, :] * ffn
    return _rmsnorm(x, final_g)
```

```python
import contextlib
import numpy as np
import concourse.bass as bass
import concourse.mybir as mybir
from concourse.bass_utils import run_bass_kernel_spmd

F32 = mybir.dt.float32
BF16 = mybir.dt.bfloat16
U8 = mybir.dt.uint8
AF = mybir.ActivationFunctionType
ALU = mybir.AluOpType

ENGINES = ("pe", "act", "dve", "pool", "sp")
L_DEPTH = 4
D = 1024
S_LEN = 4096
NE = 32
EPS = 1e-6
NCV = 68


class Op:
    __slots__ = ("eng", "fn", "deps", "needs_inc", "ev_sem", "ev_val", "is_dma", "group")

    def __init__(self, eng, fn, is_dma=False):
        self.eng = eng
        self.fn = fn
        self.deps = []
        self.needs_inc = False
        self.ev_sem = None
        self.ev_val = None
        self.is_dma = is_dma
        self.group = None


class Sched:
    def __init__(self, nc):
        self.nc = nc
        self.ops = {e: [] for e in ENGINES}
        self.res_w = {}
        self.res_r = {}
        self.sem_names = []
        self.dma_counts = {}
        self.groups = {}
        self.last_dma = {}
        self.pending = {e: [] for e in ENGINES}

    def _add_deps(self, o, reads, writes):
        deps = []
        for r in reads:
            w = self.res_w.get(r)
            if w is not None:
                deps.append((w, "raw"))
        for w_ in writes:
            w = self.res_w.get(w_)
            if w is not None:
                deps.append((w, "waw"))
            rr = self.res_r.get(w_)
            if rr:
                for rd in rr.values():
                    deps.append((rd, "war"))
        if self.pending[o.eng]:
            for d in self.pending[o.eng]:
                deps.append((d, "bar"))
            self.pending[o.eng] = []
        seen = set()
        for d, kind in deps:
            if d is o or id(d) in seen:
                continue
            if d.eng == o.eng and not d.is_dma and not o.is_dma:
                if kind != "raw" or o.eng == "pe":
                    continue
            seen.add(id(d))
            o.deps.append(d)
            d.needs_inc = True
        for r in reads:
            rr = self.res_r.setdefault(r, {})
            key = o.eng if not o.is_dma else ("dma", id(o))
            rr[key] = o
        for w_ in writes:
            self.res_w[w_] = o
            self.res_r[w_] = {}

    def op(self, eng, fn, reads=(), writes=()):
        o = Op(eng, fn)
        self._add_deps(o, reads, writes)
        self.ops[eng].append(o)
        return o

    def dma(self, eng, out, in_, sem, reads=(), writes=(), group=None, **kw):
        def fn(e):
            return e.dma_start(out=out, in_=in_, **kw)
        o = Op(eng, fn, is_dma=True)
        if sem not in self.dma_counts:
            self.dma_counts[sem] = 0
            self.sem_names.append(sem)
        self.dma_counts[sem] += 1
        o.ev_sem = sem
        o.ev_val = 16 * self.dma_counts[sem]
        o.group = group
        if group is not None:
            self.groups.setdefault(group, []).append(o)
        self._add_deps(o, reads, writes)
        self.ops[eng].append(o)
        self.last_dma[sem] = o
        return o

    def barrier(self):
        lasts = []
        for e in ENGINES:
            for o in reversed(self.ops[e]):
                if not o.is_dma:
                    lasts.append(o)
                    break
        dmas = list(self.last_dma.values())
        for e in ENGINES:
            self.pending[e] = [o for o in lasts if o.eng != e] + dmas
        self.res_w = {}
        self.res_r = {}

    def emit(self, final_wait_ops=()):
        nc = self.nc
        for d in final_wait_ops:
            d.needs_inc = True
        for e in ENGINES:
            cnt = 0
            for o in self.ops[e]:
                if o.is_dma:
                    continue
                if o.needs_inc:
                    cnt += 1
                    o.ev_sem = "eng_" + e
                    o.ev_val = cnt
        for g, lst in self.groups.items():
            tot = max(o.ev_val for o in lst)
            for o in lst:
                o.ev_val = tot
        sem_keys = ["eng_" + e for e in ENGINES] + list(self.sem_names)
        with contextlib.ExitStack() as st:
            sems = {}
            for k in sem_keys:
                sems[k] = st.enter_context(nc.semaphore(k))
            block = st.enter_context(nc.Block())

            def run(engname, eng):
                waited = {}
                for o in self.ops[engname]:
                    need = {}
                    for d in o.deps:
                        if waited.get(d.ev_sem, 0) >= d.ev_val:
                            continue
                        need[d.ev_sem] = max(need.get(d.ev_sem, 0), d.ev_val)
                    for sk, v in need.items():
                        eng.wait_ge(sems[sk], v)
                        waited[sk] = v
                    inst = o.fn(eng)
                    if o.is_dma:
                        inst.then_inc(sems[o.ev_sem], 16)
                    elif o.needs_inc:
                        inst.then_inc(sems[o.ev_sem], 1)
                if engname == "sp":
                    need = {}
                    for d in final_wait_ops:
                        need[d.ev_sem] = max(need.get(d.ev_sem, 0), d.ev_val)
                    for sk, v in need.items():
                        if waited.get(sk, 0) < v:
                            eng.wait_ge(sems[sk], v)

            @block.tensor
            def _(eng):
                run("pe", eng)

            @block.scalar
            def _(eng):
                run("act", eng)

            @block.vector
            def _(eng):
                run("dve", eng)

            @block.gpsimd
            def _(eng):
                run("pool", eng)

            @block.sync
            def _(eng):
                run("sp", eng)


class Ring:
    def __init__(self, name, aps):
        self.name = name
        self.aps = aps
        self.n = len(aps)
        self.i = 0

    def next(self):
        k = self.i % self.n
        self.i += 1
        return (self.name, k), self.aps[k], "%s%d" % (self.name, k)


def cv(l, name, idx=0):
    base = l * NCV
    off = {"cw": 0, "cb": 32, "ba": 40, "bx": 48, "lam": 56, "ps": 64}[name]
    return base + off + idx


def build_program(n_layers=L_DEPTH, stop_after=None, use_gelu_tanh=False, dbg_ne=NE, dbg_np=4, dbg_skip_mixer=False, dbg_cut=99, dbg_f=0):
    nc = bass.Bass("TRN2", target_bir_lowering=False)

    def din(name, shape, dt=F32):
        return nc.dram_tensor(name, list(shape), dt, kind="ExternalInput").ap()

    d_x = din("x", [S_LEN, D])
    d_cfm = din("cfm", [128, 8])
    d_w_ada = din("w_ada", [L_DEPTH, D, 6 * D])
    d_b_ada = din("b_ada", [L_DEPTH, 6 * D])
    d_w_in = din("w_in", [L_DEPTH, D, 4608])
    d_w_rg_a = din("w_rg_a", [L_DEPTH, 8, 128, 128])
    d_w_rg_x = din("w_rg_x", [L_DEPTH, 8, 128, 128])
    d_w_pool = din("w_pool", [L_DEPTH, 4, 128, 128])
    d_w_up_a = din("w_up_a", [L_DEPTH, D, D])
    d_w_up_b = din("w_up_b", [L_DEPTH, 512, D])
    d_w_out = din("w_out", [L_DEPTH, D, D])
    d_w_router = din("w_router", [L_DEPTH, D, NE])
    d_b_router = din("b_router", [L_DEPTH, NE])
    d_w_e_in = din("w_e_in", [L_DEPTH, NE, D, 2 * D])
    d_b1 = din("b1", [128, L_DEPTH * 512])
    d_w_e_out = din("w_e_out", [L_DEPTH, NE, D, D])
    d_b_e_out = din("b_e_out", [L_DEPTH, NE, D])
    d_ng = din("ng", [9, D])
    d_cvec = din("cvec", [128, L_DEPTH * NCV])
    d_ident = din("ident", [128, 128])
    d_cinv = din("cinv", [4, 16])
    d_xres = nc.dram_tensor("xres", [S_LEN, D], F32, kind="Internal").ap()
    d_out = nc.dram_tensor("out", [S_LEN, D], F32, kind="ExternalOutput").ap()

    xin_v = d_x.rearrange("(s p) d -> p s d", p=128)
    xres_v = d_xres.rearrange("(s p) d -> p s d", p=128)
    out_v = d_out.rearrange("(s p) d -> p s d", p=128)

    st = contextlib.ExitStack()
    ARENA = 207 * 1024
    arena = st.enter_context(nc.sbuf_tensor("arena", [128, ARENA], U8))
    ps = [st.enter_context(nc.psum_tensor("ps%d" % i, [128, 512], F32)) for i in range(8)]

    class Carver:
        def __init__(self, base, limit):
            self.off = base
            self.limit = limit

        def get(self, nbytes_free, dt, shape=None):
            nb = (nbytes_free + 31) // 32 * 32
            a = arena[:, self.off:self.off + nbytes_free].bitcast(dt)
            self.off += nb
            assert self.off <= self.limit, (self.off, self.limit)
            if shape is not None and len(shape) == 2:
                a = a.rearrange("p (a b) -> p a b", b=shape[1])
            if shape is not None and len(shape) == 3:
                a = a.rearrange("p (a b c) -> p a b c", b=shape[1], c=shape[2])
            return a

    PERS = 41 * 1024
    pc = Carver(0, PERS)
    ident = pc.get(128 * 4, F32)
    cvec = pc.get(L_DEPTH * NCV * 4, F32)
    cactT = pc.get(8 * 128 * 2, BF16, (8, 128))
    mod3 = pc.get(3 * 1024 * 4, F32, (3, 1024))
    gbc = pc.get(1024 * 4, F32)
    xs = [pc.get(1024 * 4, F32) for _ in range(2)]
    htm = [pc.get(1024 * 4, F32) for _ in range(2)]
    small = pc.get(256 * 4, F32)
    cfm = small[:, 0:8]
    csig = small[:, 8:16]
    cact = small[:, 16:24]
    m8sp = small[:, 24:32]
    hcar = small[:, 32:40]
    stat = small[:, 40:104]
    ccar = pc.get(8 * 4 * 2, BF16, (8, 4))
    pcar = pc.get(4 * 16 * 4, F32, (4, 16))
    wr32 = pc.get(8 * 32 * 4, F32, (8, 32))
    brbc = pc.get(32 * 4, F32)
    b1 = pc.get(512 * 4, F32, (32, 16))
    cinv = pc.get(4 * 16 * 4, F32, (4, 16))
    pers_end = pc.off

    S = Sched(nc)
    xs_ring = Ring("xs", xs)
    htm_ring = Ring("htm", htm)
    bb_ring = Ring("htm", [h[:, 0:512] for h in htm])
    stat_i = [0]

    def stat_col(n=1):
        k = stat_i[0]
        stat_i[0] = (k + n) % 60
        if stat_i[0] < n:
            k = 0
            stat_i[0] = n
        return k

    class Rot:
        def __init__(self, banks):
            self.banks = banks
            self.i = 0

        def next(self):
            b = self.banks[self.i % len(self.banks)]
            self.i += 1
            return b

    rot_tr = Rot([0, 1])
    rot_z = Rot([2, 3, 4])
    rot_aux = Rot([5, 6, 7])

    def PSK(b):
        return ("ps", b)

    S.dma("sp", ident, d_ident, "cst", writes=["ident"], group="cst")
    S.dma("sp", cvec, d_cvec, "cst", writes=["cvec"], group="cst")
    S.dma("sp", cfm, d_cfm, "cst", writes=["cfm"], group="cst")
    for g in range(4):
        S.dma("sp", cinv[:, g, :], d_cinv[g:g + 1, :].partition_broadcast(128), "cst", writes=[("cinv", g)], group="cst")
    S.op("act", lambda e: e.activation(out=csig, in_=cfm, func=AF.Sigmoid), reads=["cfm"], writes=["csig"])
    S.op("dve", lambda e: e.tensor_tensor(out=cact, in0=cfm, in1=csig, op=ALU.mult), reads=["cfm", "csig"], writes=["cact"])
    for k in range(8):
        S.op("dve", lambda e, k=k: e.tensor_copy(out=cactT[:, k, :], in_=cact[:, k:k + 1].to_broadcast([128, 128])),
             reads=["cact"], writes=["cactT"])

    PH0 = PERS

    def rmsnorm_rstd(xs_ap, xs_key, junk_ap, junk_key):
        c0 = stat_col(3)
        ss = stat[:, c0:c0 + 1]
        vv = stat[:, c0 + 1:c0 + 2]
        rs = stat[:, c0 + 2:c0 + 3]
        kss, kvv, krs = ("stat", c0), ("stat", c0 + 1), ("stat", c0 + 2)
        S.op("act", lambda e: e.activation(out=junk_ap, in_=xs_ap, func=AF.Square, accum_out=ss),
             reads=[xs_key], writes=[junk_key, kss])
        S.op("dve", lambda e: e.tensor_scalar(out=vv, in0=ss, scalar1=1.0 / D, scalar2=EPS, op0=ALU.mult, op1=ALU.add),
             reads=[kss], writes=[kvv])
        S.op("act", lambda e: e.activation(out=vv, in_=vv, func=AF.Sqrt), reads=[kvv], writes=[kvv])
        S.op("dve", lambda e: e.reciprocal(out=rs, in_=vv), reads=[kvv], writes=[krs])
        return rs, krs

    def load_x(layer, s, from_input=False):
        key, ap, sem = xs_ring.next()
        src = xin_v[:, s, :] if from_input else xres_v[:, s, :]
        rd = [] if from_input else [("xres", s)]
        S.dma("sp", ap, src, "ld_" + sem, reads=rd, writes=[key])
        return key, ap

    def store_x(ap, key, s):
        S.dma("sp", xres_v[:, s, :], ap, "st_" + key[0] + str(key[1]), reads=[key], writes=[("xres", s)])

    def compute_mod(l, half, wst_ring, norm_row):
        S.dma("sp", gbc, d_ng[norm_row:norm_row + 1, :].partition_broadcast(128), "gbc", writes=["gbc"])
        for nb in range(6):
            col0 = half * 3072 + nb * 512
            wk, wap, wsem = wst_ring.next()
            S.dma("pool", wap, d_w_ada[l, :, col0:col0 + 512].rearrange("(k p) n -> p k n", p=128), "w_" + wsem,
                  writes=[wk])
            bk, bap, bsem = bb_ring.next()
            S.dma("sp", bap, d_b_ada[l:l + 1, col0:col0 + 512].partition_broadcast(128), "b_" + bsem, writes=[bk])
            bank = rot_z.next()

            def mm(e, wap=wap, bank=bank):
                for k in range(8):
                    ins = e.matmul(ps[bank][:], lhsT=cactT[:, k, :], rhs=wap[:, k, :], start=(k == 0), stop=(k == 7))
                return ins
            S.op("pe", mm, reads=[wk, "cactT"], writes=[PSK(bank)])
            j, h2 = nb // 2, nb % 2
            dst = mod3[:, j, h2 * 512:(h2 + 1) * 512]
            S.op("dve", lambda e, dst=dst, bank=bank, bap=bap: e.tensor_tensor(out=dst, in0=ps[bank][:], in1=bap, op=ALU.add),
                 reads=[PSK(bank), bk], writes=[("mod3", j, h2)])
        S.op("dve", lambda e: e.scalar_tensor_tensor(out=mod3[:, 1, :], in0=mod3[:, 1, :], scalar=1.0, in1=gbc,
                                                     op0=ALU.add, op1=ALU.mult),
             reads=[("mod3", 1, 0), ("mod3", 1, 1), "gbc"], writes=[("mod3", 1, 0), ("mod3", 1, 1)])

    MOD_ALL = [("mod3", j, h) for j in range(3) for h in range(2)]

    def norm_modulate(xs_ap, xs_key):
        hk, hap, _ = htm_ring.next()
        rs, krs = rmsnorm_rstd(xs_ap, xs_key, hap, hk)
        S.op("dve", lambda e: e.scalar_tensor_tensor(out=hap, in0=xs_ap, scalar=rs, in1=mod3[:, 1, :], op0=ALU.mult, op1=ALU.mult),
             reads=[xs_key, krs, ("mod3", 1, 0), ("mod3", 1, 1)], writes=[hk])
        S.op("dve", lambda e: e.tensor_tensor(out=hap, in0=hap, in1=mod3[:, 0, :], op=ALU.add),
             reads=[hk, ("mod3", 0, 0), ("mod3", 0, 1)], writes=[hk])
        return hk, hap

    def transpose_to_fm(hk, hap, dst_fn, dst_keys, extra32=None):
        for half in range(2):
            bank = rot_tr.next()

            def tr(e, half=half, bank=bank):
                for q in range(4):
                    k = half * 4 + q
                    ins = e.transpose(ps[bank][:, q * 128:(q + 1) * 128], hap[:, k * 128:(k + 1) * 128], ident)
                return ins
            S.op("pe", tr, reads=[hk, "ident"], writes=[PSK(bank)])
            dst = dst_fn(half * 4)
            src = ps[bank][:].rearrange("p (a b) -> p a b", b=128)
            if extra32 is not None:
                d32, k32 = extra32(half * 4)
                S.op("act", lambda e, d32=d32, src=src: e.activation(out=d32, in_=src, func=AF.Copy),
                     reads=[PSK(bank)], writes=[k32])
                S.op("dve", lambda e, dst=dst, d32=d32: e.tensor_copy(out=dst, in_=d32),
                     reads=[k32], writes=[dst_keys[half]])
                continue
            eng = "act" if half == 0 else "dve"
            if eng == "act":
                S.op("act", lambda e, dst=dst, src=src: e.activation(out=dst, in_=src, func=AF.Copy),
                     reads=[PSK(bank)], writes=[dst_keys[half]])
            else:
                S.op("dve", lambda e, dst=dst, src=src: e.tensor_copy(out=dst, in_=src),
                     reads=[PSK(bank)], writes=[dst_keys[half]])

    def mixer_phase(l):
        mc = Carver(PH0, ARENA)
        wst = [mc.get(8 * 512 * 2, BF16, (8, 512)) for _ in range(3)]
        wst_ring = Ring("wst", wst)
        wrga = mc.get(8 * 128 * 2, BF16, (8, 128))
        wrgx = mc.get(8 * 128 * 2, BF16, (8, 128))
        wpool = mc.get(4 * 128 * 2, BF16, (4, 128))
        wupa = mc.get(8 * 1024 * 2, BF16, (8, 1024))
        wupb = mc.get(4 * 1024 * 2, BF16, (4, 1024))
        wout = mc.get(8 * 1024 * 2, BF16, (8, 1024))
        dg = mc.get(32 * 128 * 2, BF16, (32, 128))
        hfm = [mc.get(8 * 512 * 2, BF16, (8, 512)) for _ in range(2)]
        ylru = mc.get(8 * 512 * 2, BF16, (8, 512))
        ypool = mc.get(4 * 512 * 2, BF16, (4, 512))
        merged = mc.get(8 * 512 * 2, BF16, (8, 512))
        tmps = [mc.get(512 * 4, F32) for _ in range(12)]
        tmp_ring = Ring("tmp", tmps)
        xlb = [mc.get(516 * 2, BF16) for _ in range(2)]
        xlb_ring = Ring("xlb", xlb)
        xcb = [mc.get(512 * 2, BF16) for _ in range(2)]
        xcb_ring = Ring("xcb", xcb)
        xpb = [mc.get(528 * 4, F32) for _ in range(2)]
        xpb_ring = Ring("xpb", xpb)
        xpt = [mc.get(528 * 4, F32) for _ in range(2)]
        xpt_ring = Ring("xpt", xpt)
        plb = [mc.get(512 * 2, BF16) for _ in range(2)]
        plb_ring = Ring("plb", plb)

        gk = ("mw", l)
        S.dma("pool", wrga, d_w_rg_a[l].rearrange("h i o -> i h o"), "mw", writes=["wrga"], group=gk)
        S.dma("pool", wrgx, d_w_rg_x[l].rearrange("h i o -> i h o"), "mw", writes=["wrgx"], group=gk)
        S.dma("pool", wpool, d_w_pool[l].rearrange("h i o -> i h o"), "mw", writes=["wpool"], group=gk)
        S.dma("pool", wupa, d_w_up_a[l].rearrange("(k p) n -> p k n", p=128), "mw", writes=["wupa"], group=gk)
        S.dma("pool", wupb, d_w_up_b[l].rearrange("(k p) n -> p k n", p=128), "mw", writes=["wupb"], group=gk)
        S.dma("pool", wout, d_w_out[l].rearrange("(k p) n -> p k n", p=128), "mw", writes=["wout"], group=gk)

        compute_mod(l, 0, wst_ring, l)

        for c in range(8):
            for k in range(4):
                j = c * 4 + k
                col = cv(l, "cw", k * 8 + c)
                S.op("dve", lambda e, j=j, col=col: e.tensor_scalar(out=dg[:, j, :], in0=ident, scalar1=cvec[:, col:col + 1],
                                                                   scalar2=None, op0=ALU.mult),
                     reads=["ident", "cvec"], writes=[("dg", j)])
        lam = cvec[:, cv(l, "lam"):cv(l, "lam") + 8]
        t8 = stat[:, 60:68] if False else small[:, 104:112]
        S.op("act", lambda e: e.activation(out=t8, in_=lam, func=AF.Exp, scale=-1.0), reads=["cvec"], writes=["t8"])
        S.op("act", lambda e: e.activation(out=t8, in_=t8, func=AF.Ln, bias=1.0), reads=["t8"], writes=["t8"])
        S.op("dve", lambda e: e.tensor_scalar(out=m8sp, in0=t8, scalar1=-8.0, scalar2=None, op0=ALU.mult), reads=["t8"], writes=["m8sp"])
        S.op("dve", lambda e: e.memset(hcar, 0.0), writes=[("hcar", c) for c in range(8)])
        S.op("dve", lambda e: e.memset(ccar, 0.0), writes=[("ccar", c) for c in range(8)])
        S.op("dve", lambda e: e.memset(pcar, 0.0), writes=[("pcar", g) for g in range(4)])

        n_tiles = S_LEN // 512
        for ti in range(n_tiles):
            hf = hfm[ti % 2]
            hfk = ("hfm", ti % 2)
            for j in range(4):
                s = ti * 4 + j
                xk, xap = load_x(l, s, from_input=(l == 0))
                hk, hap = norm_modulate(xap, xk)
                transpose_to_fm(hk, hap, lambda k0, j=j, hf=hf: hf[:, k0:k0 + 4, j * 128:(j + 1) * 128],
                                [hfk + (j, 0), hfk + (j, 1)])
            hf_keys = [hfk + (j, h) for j in range(4) for h in range(2)]

            def inproj(wap, wk, q, hf=hf, hf_keys=hf_keys):
                bank = rot_z.next()

                def mm(e, wap=wap, q=q, bank=bank):
                    for k in range(8):
                        ins = e.matmul(ps[bank][:], lhsT=wap[:, k, q * 128:(q + 1) * 128], rhs=hf[:, k, :],
                                       start=(k == 0), stop=(k == 7))
                    return ins
                S.op("pe", mm, reads=[wk] + hf_keys, writes=[PSK(bank)])
                return bank

            def load_piece(col0, ncols=512, part=None, ring_slot=None):
                if ring_slot is None:
                    wk, wap, wsem = wst_ring.next()
                else:
                    wk, wap, wsem = ring_slot
                if part is None:
                    S.dma("pool", wap, d_w_in[l, :, col0:col0 + ncols].rearrange("(k p) n -> p k n", p=128), "w_" + wsem,
                          writes=[wk])
                return wk, wap, wsem

            for pc_ in range(2):
                wk, wap, _ = load_piece(1024 + pc_ * 512)
                for q in range(4):
                    c = pc_ * 4 + q
                    bank = inproj(wap, wk, q)
                    if use_gelu_tanh:
                        S.op("act", lambda e, c=c, bank=bank: e.activation(out=ylru[:, c, :], in_=ps[bank][:], func=AF.Gelu_apprx_tanh),
                             reads=[PSK(bank)], writes=[("ylru", c)])
                    else:
                        gk_, g_, _ = tmp_ring.next()
                        tk_, t_, _ = tmp_ring.next()
                        S.op("act", lambda e, g_=g_, bank=bank: e.activation(out=g_, in_=ps[bank][:], func=AF.Copy),
                             reads=[PSK(bank)], writes=[gk_])
                        S.op("dve", lambda e, g_=g_, t_=t_: e.scalar_tensor_tensor(out=t_, in0=g_, scalar=0.044715, in1=g_,
                                                                                  op0=ALU.mult, op1=ALU.mult),
                             reads=[gk_], writes=[tk_])
                        S.op("dve", lambda e, g_=g_, t_=t_: e.scalar_tensor_tensor(out=t_, in0=t_, scalar=1.0, in1=g_,
                                                                                  op0=ALU.add, op1=ALU.mult),
                             reads=[gk_, tk_], writes=[tk_])
                        S.op("act", lambda e, t_=t_: e.activation(out=t_, in_=t_, func=AF.Sigmoid, scale=1.5957691216057308),
                             reads=[tk_], writes=[tk_])
                        S.op("dve", lambda e, g_=g_, t_=t_, c=c: e.tensor_tensor(out=ylru[:, c, :], in0=g_, in1=t_, op=ALU.mult),
                             reads=[gk_, tk_], writes=[("ylru", c)])

            for pc_ in range(2):
                wk, wap, _ = load_piece(pc_ * 512)
                for q in range(4):
                    c = pc_ * 4 + q
                    bank = inproj(wap, wk, q)
                    xk_, xl_, _ = xlb_ring.next()
                    S.op("act", lambda e, xl_=xl_, c=c: e.activation(out=xl_[:, 0:3], in_=ccar[:, c, 0:3], func=AF.Copy),
                         reads=[("ccar", c)], writes=[xk_])
                    S.op("act", lambda e, xl_=xl_, bank=bank: e.activation(out=xl_[:, 3:515], in_=ps[bank][:], func=AF.Copy),
                         reads=[PSK(bank)], writes=[xk_])
                    S.op("act", lambda e, xl_=xl_, c=c: e.activation(out=ccar[:, c, 0:3], in_=xl_[:, 512:515], func=AF.Copy),
                         reads=[xk_], writes=[("ccar", c)])
                    b2 = rot_aux.next()

                    def conv(e, xl_=xl_, c=c, b2=b2):
                        for k in range(4):
                            ins = e.matmul(ps[b2][:], lhsT=dg[:, c * 4 + k, :], rhs=xl_[:, k:k + 512], start=(k == 0), stop=(k == 3))
                        return ins
                    S.op("pe", conv, reads=[xk_] + [("dg", c * 4 + k) for k in range(4)], writes=[PSK(b2)])
                    xck, xc_, _ = xcb_ring.next()
                    cb_col = cv(l, "cb", c)
                    S.op("act", lambda e, xc_=xc_, b2=b2, cb_col=cb_col: e.activation(out=xc_, in_=ps[b2][:], func=AF.Identity,
                                                                                     bias=cvec[:, cb_col:cb_col + 1]),
                         reads=[PSK(b2), "cvec"], writes=[xck])
                    b3 = rot_aux.next()
                    S.op("pe", lambda e, b3=b3, c=c, xc_=xc_: e.matmul(ps[b3][:], lhsT=wrga[:, c, :], rhs=xc_, start=True, stop=True),
                         reads=[xck, "wrga"], writes=[PSK(b3)])
                    b4 = rot_aux.next()
                    S.op("pe", lambda e, b4=b4, c=c, xc_=xc_: e.matmul(ps[b4][:], lhsT=wrgx[:, c, :], rhs=xc_, start=True, stop=True),
                         reads=[xck, "wrgx"], writes=[PSK(b4)])
                    rk, r_, _ = tmp_ring.next()
                    ik, i_, _ = tmp_ring.next()
                    ak, a_, _ = tmp_ring.next()
                    mk, m_, _ = tmp_ring.next()
                    ba_col = cv(l, "ba", c)
                    bx_col = cv(l, "bx", c)
                    S.op("act", lambda e, r_=r_, b3=b3, ba_col=ba_col: e.activation(out=r_, in_=ps[b3][:], func=AF.Sigmoid,
                                                                                   bias=cvec[:, ba_col:ba_col + 1]),
                         reads=[PSK(b3), "cvec"], writes=[rk])
                    S.op("act", lambda e, i_=i_, b4=b4, bx_col=bx_col: e.activation(out=i_, in_=ps[b4][:], func=AF.Sigmoid,
                                                                                   bias=cvec[:, bx_col:bx_col + 1]),
                         reads=[PSK(b4), "cvec"], writes=[ik])
                    S.op("act", lambda e, a_=a_, r_=r_, c=c: e.activation(out=a_, in_=r_, func=AF.Exp, scale=m8sp[:, c:c + 1]),
                         reads=[rk, "m8sp"], writes=[ak])
                    S.op("dve", lambda e, a_=a_, m_=m_: e.scalar_tensor_tensor(out=m_, in0=a_, scalar=-1.0, in1=a_,
                                                                              op0=ALU.mult, op1=ALU.mult),
                         reads=[ak], writes=[mk])
                    S.op("act", lambda e, m_=m_: e.activation(out=m_, in_=m_, func=AF.Sqrt, bias=1.0, scale=1.0),
                         reads=[mk], writes=[mk])
                    S.op("dve", lambda e, i_=i_, xc_=xc_: e.tensor_tensor(out=i_, in0=i_, in1=xc_, op=ALU.mult),
                         reads=[ik, xck], writes=[ik])
                    S.op("dve", lambda e, i_=i_, m_=m_: e.tensor_tensor(out=i_, in0=i_, in1=m_, op=ALU.mult),
                         reads=[ik, mk], writes=[ik])
                    S.op("dve", lambda e, r_=r_, a_=a_, i_=i_, c=c: e.tensor_tensor_scan(out=r_, data0=a_, data1=i_,
                                                                                        initial=hcar[:, c:c + 1],
                                                                                        op0=ALU.mult, op1=ALU.add),
                         reads=[ak, ik, ("hcar", c), rk], writes=[rk])
                    S.op("act", lambda e, r_=r_, c=c: e.activation(out=hcar[:, c:c + 1], in_=r_[:, 511:512], func=AF.Copy),
                         reads=[rk], writes=[("hcar", c)])
                    S.op("dve", lambda e, r_=r_, c=c: e.tensor_tensor(out=ylru[:, c, :], in0=ylru[:, c, :], in1=r_, op=ALU.mult),
                         reads=[rk, ("ylru", c)], writes=[("ylru", c)])

            wk, wap, _ = load_piece(2048)
            for g in range(4):
                w = 2 << g
                bank = inproj(wap, wk, g)
                pk, pb, _ = xpb_ring.next()
                S.op("act", lambda e, pb=pb, g=g: e.activation(out=pb[:, 0:15], in_=pcar[:, g, 0:15], func=AF.Copy),
                     reads=[("pcar", g)], writes=[pk])
                S.op("act", lambda e, pb=pb, bank=bank: e.activation(out=pb[:, 15:527], in_=ps[bank][:], func=AF.Copy),
                     reads=[PSK(bank)], writes=[pk])
                S.op("act", lambda e, pb=pb, g=g: e.activation(out=pcar[:, g, 0:15], in_=pb[:, 512:527], func=AF.Copy),
                     reads=[pk], writes=[("pcar", g)])
                cur, curk, lo = pb, pk, 0
                sh = 1
                for step in range(g + 1):
                    nk, nb_, _ = xpt_ring.next()
                    n = 527 - (lo + sh)
                    S.op("dve", lambda e, nb_=nb_, cur=cur, lo=lo, sh=sh, n=n: e.tensor_tensor(
                        out=nb_[:, lo + sh:527], in0=cur[:, lo + sh:527], in1=cur[:, lo:lo + n], op=ALU.add),
                        reads=[curk], writes=[nk])
                    cur, curk, lo = nb_, nk, lo + sh
                    sh *= 2
                plk, pl_, _ = plb_ring.next()
                S.op("dve", lambda e, pl_=pl_, cur=cur, pb=pb, w=w: e.scalar_tensor_tensor(
                    out=pl_, in0=cur[:, 15:527], scalar=1.0 / w, in1=pb[:, 15:527], op0=ALU.mult, op1=ALU.subtract),
                    reads=[curk, pk], writes=[plk])
                if ti == 0:
                    tk_, t_, _ = tmp_ring.next()
                    S.op("dve", lambda e, t_=t_, cur=cur, g=g: e.tensor_tensor(out=t_[:, 0:16], in0=cur[:, 15:31], in1=cinv[:, g, :], op=ALU.mult),
                         reads=[curk, ("cinv", g)], writes=[tk_])
                    S.op("dve", lambda e, t_=t_, pl_=pl_, pb=pb: e.tensor_tensor(out=pl_[:, 0:16], in0=t_[:, 0:16], in1=pb[:, 15:31], op=ALU.subtract),
                         reads=[tk_, pk, plk], writes=[plk])
                b2 = rot_aux.next()
                S.op("pe", lambda e, b2=b2, g=g, pl_=pl_: e.matmul(ps[b2][:], lhsT=wpool[:, g, :], rhs=pl_, start=True, stop=True),
                     reads=[plk, "wpool"], writes=[PSK(b2)])
                psc = cv(l, "ps", g)
                S.op("act", lambda e, g=g, b2=b2, psc=psc: e.activation(out=ypool[:, g, :], in_=ps[b2][:], func=AF.Copy,
                                                                         scale=cvec[:, psc:psc + 1]),
                     reads=[PSK(b2), "cvec"], writes=[("ypool", g)])

            for qq in range(4):
                wk, wap, wsem = wst_ring.next()
                cA = 2560 + qq * 256
                cB = 2560 + 1024 + qq * 256
                S.dma("pool", wap[:, :, 0:256], d_w_in[l, :, cA:cA + 256].rearrange("(k p) n -> p k n", p=128), "w_" + wsem,
                      writes=[wk])
                S.dma("pool", wap[:, :, 256:512], d_w_in[l, :, cB:cB + 256].rearrange("(k p) n -> p k n", p=128), "w_" + wsem,
                      writes=[wk])
                gts = []
                for q in range(4):
                    bank = rot_z.next()

                    def mm(e, wap=wap, q=q, bank=bank, hf=hf):
                        for k in range(8):
                            ins = e.matmul(ps[bank][:], lhsT=wap[:, k, q * 128:(q + 1) * 128], rhs=hf[:, k, :],
                                           start=(k == 0), stop=(k == 7))
                        return ins
                    S.op("pe", mm, reads=[wk] + hf_keys, writes=[PSK(bank)])
                    gk_, g_, _ = tmp_ring.next()
                    S.op("act", lambda e, g_=g_, bank=bank: e.activation(out=g_, in_=ps[bank][:], func=AF.Sigmoid),
                         reads=[PSK(bank)], writes=[gk_])
                    gts.append((gk_, g_))
                for cc in range(2):
                    c = qq * 2 + cc
                    g0k, g0 = gts[cc]
                    g1k, g1 = gts[2 + cc]
                    ba_ = rot_aux.next()

                    def mma(e, c=c, ba_=ba_):
                        for k in range(8):
                            ins = e.matmul(ps[ba_][:], lhsT=wupa[:, k, c * 128:(c + 1) * 128], rhs=ylru[:, k, :],
                                           start=(k == 0), stop=(k == 7))
                        return ins
                    S.op("pe", mma, reads=["wupa"] + [("ylru", k) for k in range(8)], writes=[PSK(ba_)])
                    bb_ = rot_aux.next()

                    def mmb(e, c=c, bb_=bb_):
                        for k in range(4):
                            ins = e.matmul(ps[bb_][:], lhsT=wupb[:, k, c * 128:(c + 1) * 128], rhs=ypool[:, k, :],
                                           start=(k == 0), stop=(k == 3))
                        return ins
                    S.op("pe", mmb, reads=["wupb"] + [("ypool", k) for k in range(4)], writes=[PSK(bb_)])
                    S.op("dve", lambda e, g0=g0, ba_=ba_: e.tensor_tensor(out=g0, in0=g0, in1=ps[ba_][:], op=ALU.mult),
                         reads=[g0k, PSK(ba_)], writes=[g0k])
                    S.op("dve", lambda e, g1=g1, bb_=bb_: e.tensor_tensor(out=g1, in0=g1, in1=ps[bb_][:], op=ALU.mult),
                         reads=[g1k, PSK(bb_)], writes=[g1k])
                    S.op("dve", lambda e, g0=g0, g1=g1, c=c: e.tensor_tensor(out=merged[:, c, :], in0=g0, in1=g1, op=ALU.add),
                         reads=[g0k, g1k], writes=[("merged", c)])

            for j in range(4):
                s = ti * 4 + j
                xk, xap = load_x(l, s, from_input=(l == 0))
                for hh in range(2):
                    bank = rot_z.next()

                    def mmo(e, j=j, hh=hh, bank=bank):
                        for k in range(8):
                            ins = e.matmul(ps[bank][:], lhsT=merged[:, k, j * 128:(j + 1) * 128],
                                           rhs=wout[:, k, hh * 512:(hh + 1) * 512], start=(k == 0), stop=(k == 7))
                        return ins
                    S.op("pe", mmo, reads=["wout"] + [("merged", k) for k in range(8)], writes=[PSK(bank)])
                    tk_, t_, _ = tmp_ring.next()
                    S.op("dve", lambda e, t_=t_, bank=bank, hh=hh: e.tensor_tensor(out=t_, in0=ps[bank][:],
                                                                                  in1=mod3[:, 2, hh * 512:(hh + 1) * 512], op=ALU.mult),
                         reads=[PSK(bank), ("mod3", 2, hh)], writes=[tk_])
                    S.op("dve", lambda e, t_=t_, xap=xap, hh=hh: e.tensor_tensor(out=xap[:, hh * 512:(hh + 1) * 512],
                                                                                in0=xap[:, hh * 512:(hh + 1) * 512], in1=t_, op=ALU.add),
                         reads=[tk_, xk], writes=[xk])
                store_x(xap, xk, s)

    def moe_phase(l, last):
        mc = Carver(PH0, ARENA)
        wex = [mc.get(8 * 1024 * 2, BF16, (8, 1024)) for _ in range(5)]
        wex_ring = Ring("wex", wex)
        wst_ring = Ring("wex", [w_[:, :, 0:512] for w_ in wex])
        hfm = mc.get(8 * 1024 * 2, BF16, (8, 1024))
        yacc = mc.get(8 * 1024 * 4, F32, (8, 1024))
        actb = [mc.get(8 * 512 * 2, BF16, (8, 512)) for _ in range(2)]
        hT32 = mc.get(8 * 128 * 4, F32, (8, 128))
        gates = mc.get(8 * 32 * 4, F32, (8, 32))
        lgb = [mc.get(32 * 4, F32) for _ in range(2)]
        lg_ring = Ring("lg", lgb)
        emb = [mc.get(32 * 4, F32) for _ in range(2)]
        em_ring = Ring("em", emb)
        mx8 = [mc.get(8 * 4, F32) for _ in range(2)]
        mx_ring = Ring("mx", mx8)
        gpad = mc.get(128 * 4, F32)
        gT = [mc.get(128 * 4, F32) for _ in range(2)]
        gT_ring = Ring("gT", gT)
        beo = gbc
        tb0 = mc.off
        tmps = [mc.get(512 * 4, F32) for _ in range(6)]
        tmp_ring = Ring("tmp", tmps)
        tbig = arena[:, tb0:tb0 + 4096].bitcast(F32)
        TBK = [("tmp", 0), ("tmp", 1)]

        compute_mod(l, 1, wst_ring, 4 + l)
        S.op("dve", lambda e: e.memset(gpad, 0.0), writes=["gpad"])
        gk = ("moew", l)
        S.dma("sp", wr32, d_w_router[l].rearrange("(k p) n -> p k n", p=128), "mwc", writes=["wr32"], group=gk)
        S.dma("sp", brbc, d_b_router[l:l + 1, :].partition_broadcast(128), "mwc", writes=["brbc"], group=gk)
        S.dma("sp", b1, d_b1[:, l * 512:(l + 1) * 512].rearrange("p (a b) -> p a b", b=16), "mwc", writes=["b1"], group=gk)
        S.dma("sp", beo[0:32, :], d_b_e_out[l], "gbc", writes=["gbc"])
        S.op("dve", lambda e: e.tensor_scalar(out=b1[:, :, 8:16], in0=b1[:, :, 8:16], scalar1=1.0, scalar2=None, op0=ALU.add),
             reads=["b1"], writes=["b1"])

        rot_gu = Rot([2, 3, 4])
        rot_o = Rot([5, 6])
        MISC = 7

        n_pass = dbg_np
        for p in range(n_pass):
            for j in range(8 if dbg_cut > 0 else 0):
                s = p * 8 + j
                xk, xap = load_x(l, s, from_input=dbg_skip_mixer)
                hk, hap = norm_modulate(xap, xk)
                transpose_to_fm(hk, hap, lambda k0, j=j: hfm[:, k0:k0 + 4, j * 128:(j + 1) * 128],
                                [("hfm", j, 0), ("hfm", j, 1)],
                                extra32=(None if (dbg_f & 1) else (lambda k0: (hT32[:, k0:k0 + 4, :], ("hT32", k0 // 4)))))

                if dbg_cut <= 1:
                    continue

                def rmm(e):
                    for k in range(8):
                        ins = e.matmul(ps[MISC][:, 0:32], lhsT=hT32[:, k, :], rhs=wr32[:, k, :], start=(k == 0), stop=(k == 7))
                    return ins
                S.op("pe", rmm, reads=[("hT32", 0), ("hT32", 1), "wr32"], writes=[PSK(MISC)])
                lk, lg, _ = lg_ring.next()
                ek, em, _ = em_ring.next()
                mk_, mx, _ = mx_ring.next()
                S.op("dve", lambda e, lg=lg: e.tensor_tensor(out=lg, in0=ps[MISC][:, 0:32], in1=brbc, op=ALU.add),
                     reads=[PSK(MISC), "brbc"], writes=[lk])
                if dbg_cut <= 2:
                    continue
                S.op("dve", lambda e, lg=lg, mx=mx: e.max(out=mx, in_=lg), reads=[lk], writes=[mk_])
                if dbg_cut <= 3:
                    continue
                c0 = stat_col(3)
                negm = stat[:, c0:c0 + 1]
                den = stat[:, c0 + 1:c0 + 2]
                rden = stat[:, c0 + 2:c0 + 3]
                S.op("dve", lambda e, mx=mx, negm=negm: e.tensor_scalar(out=negm, in0=mx[:, 0:1], scalar1=-1.0, scalar2=None, op0=ALU.mult),
                     reads=[mk_], writes=[("stat", c0)])
                S.op("act", lambda e, em=em, lg=lg, negm=negm: e.activation(out=em, in_=lg, func=AF.Exp, bias=negm, scale=1.0),
                     reads=[lk, ("stat", c0)], writes=[ek])
                S.op("dve", lambda e, lg=lg, mx=mx: e.tensor_scalar(out=lg, in0=lg, scalar1=mx[:, 3:4], scalar2=None, op0=ALU.is_ge),
                     reads=[lk, mk_, ek], writes=[lk])
                S.op("dve", lambda e, em=em, lg=lg, den=den: e.scalar_tensor_tensor(out=em, in0=em, scalar=1.0, in1=lg,
                                                                                   op0=ALU.mult, op1=ALU.mult, accum_out=den),
                     reads=[ek, lk], writes=[ek, ("stat", c0 + 1)])
                S.op("dve", lambda e, den=den, rden=rden: e.reciprocal(out=rden, in_=den), reads=[("stat", c0 + 1)], writes=[("stat", c0 + 2)])
                S.op("dve", lambda e, em=em, rden=rden, j=j: e.tensor_scalar(out=gates[:, j, :], in0=em, scalar1=rden, scalar2=None, op0=ALU.mult),
                     reads=[ek, ("stat", c0 + 2)], writes=[("gates", j)])
                if dbg_cut <= 4:
                    continue
                S.op("dve", lambda e, j=j: e.tensor_copy(out=gpad[:, 0:32], in_=gates[:, j, :]),
                     reads=[("gates", j)], writes=["gpad"])
                S.op("pe", lambda e: e.transpose(ps[MISC][:, 128:256], gpad, ident),
                     reads=["gpad", "ident"], writes=[PSK(MISC)])
                gtk, gt, _ = gT_ring.next()
                S.op("act", lambda e, gt=gt: e.activation(out=gt[0:32, :], in_=ps[MISC][0:32, 128:256], func=AF.Copy),
                     reads=[PSK(MISC)], writes=[gtk])
                if dbg_cut <= 5:
                    continue
                for hh in range(2):
                    bo = rot_o.next()
                    S.op("pe", lambda e, gt=gt, hh=hh, bo=bo: e.matmul(ps[bo][:], lhsT=gt[0:32, :], rhs=beo[0:32, hh * 512:(hh + 1) * 512],
                                                                      start=True, stop=True),
                         reads=[gtk, "gbc"], writes=[PSK(bo)])
                    S.op("act", lambda e, j=j, hh=hh, bo=bo: e.activation(out=yacc[:, j, hh * 512:(hh + 1) * 512], in_=ps[bo][:], func=AF.Copy),
                         reads=[PSK(bo)], writes=[("yacc", j, hh)])

            hf_keys = [[("hfm", g * 4 + j4, h) for j4 in range(4) for h in range(2)] for g in range(2)]
            for ex in range(dbg_ne):
                wgk, wg, wgs = wex_ring.next()
                S.dma("pool", wg, d_w_e_in[l, ex, :, 0:1024].rearrange("(k p) n -> p k n", p=128), "e_" + wgs, writes=[wgk])
                wuk, wu, wus = wex_ring.next()
                S.dma("pool", wu, d_w_e_in[l, ex, :, 1024:2048].rearrange("(k p) n -> p k n", p=128), "e_" + wus, writes=[wuk])
                wok, wo, wos = wex_ring.next()
                S.dma("pool", wo, d_w_e_out[l, ex].rearrange("(k p) n -> p k n", p=128), "e_" + wos, writes=[wok])
                for g in range(2):
                    ab = actb[g]
                    for jp in range(8):
                        bg = rot_gu.next()

                        def mg(e, wg=wg, jp=jp, bg=bg, g=g):
                            for k in range(8):
                                ins = e.matmul(ps[bg][:], lhsT=wg[:, k, jp * 128:(jp + 1) * 128], rhs=hfm[:, k, g * 512:(g + 1) * 512],
                                               start=(k == 0), stop=(k == 7))
                            return ins
                        S.op("pe", mg, reads=[wgk] + hf_keys[g], writes=[PSK(bg)])
                        bu = rot_gu.next()

                        def mu(e, wu=wu, jp=jp, bu=bu, g=g):
                            for k in range(8):
                                ins = e.matmul(ps[bu][:], lhsT=wu[:, k, jp * 128:(jp + 1) * 128], rhs=hfm[:, k, g * 512:(g + 1) * 512],
                                               start=(k == 0), stop=(k == 7))
                            return ins
                        S.op("pe", mu, reads=[wuk] + hf_keys[g], writes=[PSK(bu)])
                        gck, gc, _ = tmp_ring.next()
                        sgk, sg, _ = tmp_ring.next()
                        uck, uc, _ = tmp_ring.next()
                        S.op("dve", lambda e, gc=gc, bg=bg, ex=ex, jp=jp: e.tensor_scalar(out=gc, in0=ps[bg][:], scalar1=b1[:, ex, jp:jp + 1],
                                                                                         scalar2=7.0, op0=ALU.add, op1=ALU.min),
                             reads=[PSK(bg), "b1"], writes=[gck])
                        S.op("act", lambda e, sg=sg, gc=gc: e.activation(out=sg, in_=gc, func=AF.Sigmoid, scale=1.702),
                             reads=[gck], writes=[sgk])
                        S.op("dve", lambda e, uc=uc, bu=bu, ex=ex, jp=jp: e.tensor_scalar(out=uc, in0=ps[bu][:], scalar1=b1[:, ex, 8 + jp:9 + jp],
                                                                                         scalar2=8.0, op0=ALU.add, op1=ALU.min),
                             reads=[PSK(bu), "b1"], writes=[uck])
                        S.op("dve", lambda e, gc=gc, sg=sg: e.tensor_tensor(out=gc, in0=gc, in1=sg, op=ALU.mult),
                             reads=[gck, sgk], writes=[gck])
                        S.op("dve", lambda e, ab=ab, jp=jp, uc=uc, gc=gc: e.scalar_tensor_tensor(out=ab[:, jp, :], in0=uc, scalar=-6.0, in1=gc,
                                                                                                op0=ALU.max, op1=ALU.mult),
                             reads=[uck, gck], writes=[("act", g, jp)])
                for g in range(2):
                    ab = actb[g]
                    for j4 in range(4):
                        js = g * 4 + j4
                        for hh in range(2):
                            bo = rot_o.next()

                            def mo(e, ab=ab, j4=j4, hh=hh, bo=bo, wo=wo):
                                for k in range(8):
                                    ins = e.matmul(ps[bo][:], lhsT=ab[:, k, j4 * 128:(j4 + 1) * 128], rhs=wo[:, k, hh * 512:(hh + 1) * 512],
                                                   start=(k == 0), stop=(k == 7))
                                return ins
                            S.op("pe", mo, reads=[wok] + [("act", g, k) for k in range(8)], writes=[PSK(bo)])
                            S.op("dve", lambda e, js=js, hh=hh, bo=bo, ex=ex: e.scalar_tensor_tensor(
                                out=yacc[:, js, hh * 512:(hh + 1) * 512], in0=ps[bo][:], scalar=gates[:, js, ex:ex + 1],
                                in1=yacc[:, js, hh * 512:(hh + 1) * 512], op0=ALU.mult, op1=ALU.add),
                                reads=[PSK(bo), ("gates", js), ("yacc", js, hh)], writes=[("yacc", js, hh)])
            for j in range(8 if (dbg_cut > 0 and not (dbg_f & 2)) else 0):
                s = p * 8 + j
                xk, xap = load_x(l, s, from_input=dbg_skip_mixer)
                S.op("dve", lambda e, j=j: e.tensor_tensor(out=tbig, in0=yacc[:, j, :], in1=mod3[:, 2, :], op=ALU.mult),
                     reads=[("yacc", j, 0), ("yacc", j, 1), ("mod3", 2, 0), ("mod3", 2, 1)], writes=TBK)
                S.op("dve", lambda e, xap=xap: e.tensor_tensor(out=xap, in0=xap, in1=tbig, op=ALU.add),
                     reads=TBK + [xk], writes=[xk])
                store_x(xap, xk, s)

    final_ops = []
    for l in range(n_layers):
        if not dbg_skip_mixer:
            mixer_phase(l)
            S.barrier()
        if stop_after == ("mixer", l):
            break
        moe_phase(l, last=(l == n_layers - 1 and stop_after is None))
        S.barrier()
        if stop_after == ("moe", l):
            break
    if stop_after is None:
        S.dma("sp", gbc, d_ng[8:9, :].partition_broadcast(128), "gbc", writes=["gbc"])
        for s in range(S_LEN // 128):
            xk, xap = load_x(1, s)
            hk, hap, _ = htm_ring.next()
            rs, krs = rmsnorm_rstd(xap, xk, hap, hk)
            S.op("dve", lambda e, xap=xap, rs=rs: e.scalar_tensor_tensor(out=xap, in0=xap, scalar=rs, in1=gbc, op0=ALU.mult, op1=ALU.mult),
                 reads=[xk, krs, "gbc"], writes=[xk])
            o = S.dma("sp", out_v[:, s, :], xap, "st_" + xk[0] + str(xk[1]), reads=[xk], writes=[("out", s)])
            final_ops.append(o)
    if not final_ops:
        for s in range(32):
            xk, xap = load_x(1, s)
            o = S.dma("sp", out_v[:, s, :], xap, "st_" + xk[0] + str(xk[1]), reads=[xk], writes=[("out", s)])
            final_ops.append(o)
    S.emit(final_wait_ops=final_ops)
    st.close()
    return nc


def make_in_maps(inp):
    f = lambda a: np.ascontiguousarray(np.asarray(a, dtype=np.float32))
    L = L_DEPTH
    cvec = np.zeros((128, L * NCV), np.float32)

    def fm(v):
        return np.asarray(v, np.float32).reshape(-1, 128).T

    for l in range(L):
        base = l * NCV
        for k in range(4):
            cvec[:, base + k * 8: base + k * 8 + 8] = fm(inp["conv_w"][l, k])
        cvec[:, base + 32: base + 40] = fm(inp["conv_b"][l])
        cvec[:, base + 40: base + 48] = fm(inp["b_rg_a"][l])
        cvec[:, base + 48: base + 56] = fm(inp["b_rg_x"][l])
        cvec[:, base + 56: base + 64] = fm(inp["lru_lambda"][l])
        cvec[:, base + 64: base + 68] = fm(inp["pool_scale"][l])
    b1 = np.zeros((128, L * 512), np.float32)
    be = np.asarray(inp["b_e_in"], np.float32)
    for l in range(L):
        b1[:, l * 512:(l + 1) * 512] = be[l].reshape(NE, 16, 128).transpose(2, 0, 1).reshape(128, 512)
    ng = np.concatenate([np.asarray(inp["norm1_g"], np.float32), np.asarray(inp["norm2_g"], np.float32),
                         np.asarray(inp["final_g"], np.float32)[None, :]], axis=0)
    ident = np.eye(128, dtype=np.float32)
    t = np.arange(512, dtype=np.float32)
    cinv = np.stack([1.0 / np.minimum(t[:16] + 1.0, float(w)) for w in (2, 4, 8, 16)]).astype(np.float32)
    shared = dict(
        w_ada=f(inp["w_ada"]), b_ada=f(inp["b_ada"]), w_in=f(inp["w_in"]), w_rg_a=f(inp["w_rg_a"]), w_rg_x=f(inp["w_rg_x"]),
        w_pool=f(inp["w_pool"]), w_up_a=f(inp["w_up_a"]), w_up_b=f(inp["w_up_b"]), w_out=f(inp["w_out"]),
        w_router=f(inp["w_router"]), b_router=f(inp["b_router"]), w_e_in=f(inp["w_e_in"]), b1=b1,
        w_e_out=f(inp["w_e_out"]), b_e_out=f(inp["b_e_out"]), ng=f(ng), cvec=cvec, ident=ident, cinv=cinv)
    x = f(inp["x"])
    c = np.asarray(inp["c"], np.float32)
    maps = []
    for b in range(x.shape[0]):
        m = dict(shared)
        m["x"] = x[b]
        m["cfm"] = np.ascontiguousarray(fm(c[b]))
        maps.append(m)
    return maps


_CACHE = {}


def kernel(**inputs):
    maps = make_in_maps(inputs)
    if "nc" not in _CACHE:
        _CACHE["nc"] = build_program()
    nc = _CACHE["nc"]
    res = run_bass_kernel_spmd(nc, maps, core_ids=list(range(len(maps))))
    out = np.stack([np.asarray(r["out"], dtype=np.float32) for r in res.results], axis=0)
    return out
```

```python
import contextlib
import numpy as np
import concourse.bass as bass
import concourse.mybir as mybir
from concourse.bass_utils import run_bass_kernel_spmd

F32 = mybir.dt.float32
BF16 = mybir.dt.bfloat16
U8 = mybir.dt.uint8
AF = mybir.ActivationFunctionType
ALU = mybir.AluOpType

ENGINES = ("pe", "act", "dve", "pool", "sp")
L_DEPTH = 4
D = 1024
S_LEN = 4096
NE = 32
EPS = 1e-6
NCV = 68


class Op:
    __slots__ = ("eng", "fn", "deps", "needs_inc", "ev_sem", "ev_val", "is_dma", "group")

    def __init__(self, eng, fn, is_dma=False):
        self.eng = eng
        self.fn = fn
        self.deps = []
        self.needs_inc = False
        self.ev_sem = None
        self.ev_val = None
        self.is_dma = is_dma
        self.group = None


class Sched:
    def __init__(self, nc):
        self.nc = nc
        self.ops = {e: [] for e in ENGINES}
        self.res_w = {}
        self.res_r = {}
        self.sem_names = []
        self.dma_counts = {}
        self.groups = {}
        self.last_dma = {}
        self.pending = {e: [] for e in ENGINES}

    def _add_deps(self, o, reads, writes):
        deps = []
        for r in reads:
            w = self.res_w.get(r)
            if w is not None:
                deps.append((w, "raw"))
        for w_ in writes:
            w = self.res_w.get(w_)
            if w is not None:
                deps.append((w, "waw"))
            rr = self.res_r.get(w_)
            if rr:
                for rd in rr.values():
                    deps.append((rd, "war"))
        if self.pending[o.eng]:
            for d in self.pending[o.eng]:
                deps.append((d, "bar"))
            self.pending[o.eng] = []
        seen = set()
        for d, kind in deps:
            if d is o or id(d) in seen:
                continue
            if d.eng == o.eng and not d.is_dma and not o.is_dma:
                if kind != "raw" or o.eng == "pe":
                    continue
            seen.add(id(d))
            o.deps.append(d)
            d.needs_inc = True
        for r in reads:
            rr = self.res_r.setdefault(r, {})
            key = o.eng if not o.is_dma else ("dma", id(o))
            rr[key] = o
        for w_ in writes:
            self.res_w[w_] = o
            self.res_r[w_] = {}

    def op(self, eng, fn, reads=(), writes=()):
        o = Op(eng, fn)
        self._add_deps(o, reads, writes)
        self.ops[eng].append(o)
        return o

    def dma(self, eng, out, in_, sem, reads=(), writes=(), group=None, **kw):
        def fn(e):
            return e.dma_start(out=out, in_=in_, **kw)
        o = Op(eng, fn, is_dma=True)
        if sem not in self.dma_counts:
            self.dma_counts[sem] = 0
            self.sem_names.append(sem)
        self.dma_counts[sem] += 1
        o.ev_sem = sem
        o.ev_val = 16 * self.dma_counts[sem]
        o.group = group
        if group is not None:
            self.groups.setdefault(group, []).append(o)
        self._add_deps(o, reads, writes)
        self.ops[eng].append(o)
        self.last_dma[sem] = o
        return o

    def barrier(self):
        lasts = []
        for e in ENGINES:
            for o in reversed(self.ops[e]):
                if not o.is_dma:
                    lasts.append(o)
                    break
        dmas = list(self.last_dma.values())
        for e in ENGINES:
            self.pending[e] = [o for o in lasts if o.eng != e] + dmas
        self.res_w = {}
        self.res_r = {}

    def emit(self, final_wait_ops=()):
        nc = self.nc
        for d in final_wait_ops:
            d.needs_inc = True
        for e in ENGINES:
            cnt = 0
            for o in self.ops[e]:
                if o.is_dma:
                    continue
                if o.needs_inc:
                    cnt += 1
                    o.ev_sem = "eng_" + e
                    o.ev_val = cnt
        for g, lst in self.groups.items():
            tot = max(o.ev_val for o in lst)
            for o in lst:
                o.ev_val = tot
        sem_keys = ["eng_" + e for e in ENGINES] + list(self.sem_names)
        with contextlib.ExitStack() as st:
            sems = {}
            for k in sem_keys:
                sems[k] = st.enter_context(nc.semaphore(k))
            block = st.enter_context(nc.Block())

            def run(engname, eng):
                waited = {}
                for o in self.ops[engname]:
                    need = {}
                    for d in o.deps:
                        if waited.get(d.ev_sem, 0) >= d.ev_val:
                            continue
                        need[d.ev_sem] = max(need.get(d.ev_sem, 0), d.ev_val)
                    for sk, v in need.items():
                        eng.wait_ge(sems[sk], v)
                        waited[sk] = v
                    inst = o.fn(eng)
                    if o.is_dma:
                        inst.then_inc(sems[o.ev_sem], 16)
                    elif o.needs_inc:
                        inst.then_inc(sems[o.ev_sem], 1)
                if engname == "sp":
                    need = {}
                    for d in final_wait_ops:
                        need[d.ev_sem] = max(need.get(d.ev_sem, 0), d.ev_val)
                    for sk, v in need.items():
                        if waited.get(sk, 0) < v:
                            eng.wait_ge(sems[sk], v)

            @block.tensor
            def _(eng):
                run("pe", eng)

            @block.scalar
            def _(eng):
                run("act", eng)

            @block.vector
            def _(eng):
                run("dve", eng)

            @block.gpsimd
            def _(eng):
                run("pool", eng)

            @block.sync
            def _(eng):
                run("sp", eng)


class Ring:
    def __init__(self, name, aps):
        self.name = name
        self.aps = aps
        self.n = len(aps)
        self.i = 0

    def next(self):
        k = self.i % self.n
        self.i += 1
        return (self.name, k), self.aps[k], "%s%d" % (self.name, k)


def cv(l, name, idx=0):
    base = l * NCV
    off = {"cw": 0, "cb": 32, "ba": 40, "bx": 48, "lam": 56, "ps": 64}[name]
    return base + off + idx


def build_program(n_layers=L_DEPTH, stop_after=None, use_gelu_tanh=False, dbg_ne=NE, dbg_np=4, dbg_skip_mixer=False, dbg_cut=99, dbg_f=0):
    nc = bass.Bass("TRN2", target_bir_lowering=False)

    def din(name, shape, dt=F32):
        return nc.dram_tensor(name, list(shape), dt, kind="ExternalInput").ap()

    d_x = din("x", [S_LEN, D])
    d_cfm = din("cfm", [128, 8])
    d_w_ada = din("w_ada", [L_DEPTH, D, 6 * D])
    d_b_ada = din("b_ada", [L_DEPTH, 6 * D])
    d_w_in = din("w_in", [L_DEPTH, D, 4608])
    d_w_rg_a = din("w_rg_a", [L_DEPTH, 8, 128, 128])
    d_w_rg_x = din("w_rg_x", [L_DEPTH, 8, 128, 128])
    d_w_pool = din("w_pool", [L_DEPTH, 4, 128, 128])
    d_w_up_a = din("w_up_a", [L_DEPTH, D, D])
    d_w_up_b = din("w_up_b", [L_DEPTH, 512, D])
    d_w_out = din("w_out", [L_DEPTH, D, D])
    d_w_router = din("w_router", [L_DEPTH, D, NE])
    d_b_router = din("b_router", [L_DEPTH, NE])
    d_w_e_in = din("w_e_in", [L_DEPTH, NE, D, 2 * D])
    d_b1 = din("b1", [128, L_DEPTH * 512])
    d_w_e_out = din("w_e_out", [L_DEPTH, NE, D, D])
    d_b_e_out = din("b_e_out", [L_DEPTH, NE, D])
    d_ng = din("ng", [9, D])
    d_cvec = din("cvec", [128, L_DEPTH * NCV])
    d_ident = din("ident", [128, 128])
    d_cinv = din("cinv", [4, 16])
    d_xres = nc.dram_tensor("xres", [S_LEN, D], F32, kind="Internal").ap()
    d_out = nc.dram_tensor("out", [S_LEN, D], F32, kind="ExternalOutput").ap()

    xin_v = d_x.rearrange("(s p) d -> p s d", p=128)
    xres_v = d_xres.rearrange("(s p) d -> p s d", p=128)
    out_v = d_out.rearrange("(s p) d -> p s d", p=128)

    st = contextlib.ExitStack()
    ARENA = 207 * 1024
    arena = st.enter_context(nc.sbuf_tensor("arena", [128, ARENA], U8))
    ps = [st.enter_context(nc.psum_tensor("ps%d" % i, [128, 512], F32)) for i in range(8)]

    class Carver:
        def __init__(self, base, limit):
            self.off = base
            self.limit = limit

        def get(self, nbytes_free, dt, shape=None):
            nb = (nbytes_free + 31) // 32 * 32
            a = arena[:, self.off:self.off + nbytes_free].bitcast(dt)
            self.off += nb
            assert self.off <= self.limit, (self.off, self.limit)
            if shape is not None and len(shape) == 2:
                a = a.rearrange("p (a b) -> p a b", b=shape[1])
            if shape is not None and len(shape) == 3:
                a = a.rearrange("p (a b c) -> p a b c", b=shape[1], c=shape[2])
            return a

    PERS = 41 * 1024
    pc = Carver(0, PERS)
    ident = pc.get(128 * 4, F32)
    cvec = pc.get(L_DEPTH * NCV * 4, F32)
    cactT = pc.get(8 * 128 * 2, BF16, (8, 128))
    mod3 = pc.get(3 * 1024 * 4, F32, (3, 1024))
    gbc = pc.get(1024 * 4, F32)
    xs = [pc.get(1024 * 4, F32) for _ in range(2)]
    htm = [pc.get(1024 * 4, F32) for _ in range(2)]
    small = pc.get(256 * 4, F32)
    cfm = small[:, 0:8]
    csig = small[:, 8:16]
    cact = small[:, 16:24]
    m8sp = small[:, 24:32]
    hcar = small[:, 32:40]
    stat = small[:, 40:104]
    ccar = pc.get(8 * 4 * 2, BF16, (8, 4))
    pcar = pc.get(4 * 16 * 4, F32, (4, 16))
    wr32 = pc.get(8 * 32 * 4, F32, (8, 32))
    brbc = pc.get(32 * 4, F32)
    b1 = pc.get(512 * 4, F32, (32, 16))
    cinv = pc.get(4 * 16 * 4, F32, (4, 16))
    pers_end = pc.off

    S = Sched(nc)
    xs_ring = Ring("xs", xs)
    htm_ring = Ring("htm", htm)
    bb_ring = Ring("htm", [h[:, 0:512] for h in htm])
    stat_i = [0]

    def stat_col(n=1):
        k = stat_i[0]
        stat_i[0] = (k + n) % 60
        if stat_i[0] < n:
            k = 0
            stat_i[0] = n
        return k

    class Rot:
        def __init__(self, banks):
            self.banks = banks
            self.i = 0

        def next(self):
            b = self.banks[self.i % len(self.banks)]
            self.i += 1
            return b

    rot_tr = Rot([0, 1])
    rot_z = Rot([2, 3, 4])
    rot_aux = Rot([5, 6, 7])

    def PSK(b):
        return ("ps", b)

    S.dma("sp", ident, d_ident, "cst", writes=["ident"], group="cst")
    S.dma("sp", cvec, d_cvec, "cst", writes=["cvec"], group="cst")
    S.dma("sp", cfm, d_cfm, "cst", writes=["cfm"], group="cst")
    for g in range(4):
        S.dma("sp", cinv[:, g, :], d_cinv[g:g + 1, :].partition_broadcast(128), "cst", writes=[("cinv", g)], group="cst")
    S.op("act", lambda e: e.activation(out=csig, in_=cfm, func=AF.Sigmoid), reads=["cfm"], writes=["csig"])
    S.op("dve", lambda e: e.tensor_tensor(out=cact, in0=cfm, in1=csig, op=ALU.mult), reads=["cfm", "csig"], writes=["cact"])
    for k in range(8):
        S.op("dve", lambda e, k=k: e.tensor_copy(out=cactT[:, k, :], in_=cact[:, k:k + 1].to_broadcast([128, 128])),
             reads=["cact"], writes=["cactT"])

    PH0 = PERS

    def rmsnorm_rstd(xs_ap, xs_key, junk_ap, junk_key):
        c0 = stat_col(3)
        ss = stat[:, c0:c0 + 1]
        vv = stat[:, c0 + 1:c0 + 2]
        rs = stat[:, c0 + 2:c0 + 3]
        kss, kvv, krs = ("stat", c0), ("stat", c0 + 1), ("stat", c0 + 2)
        S.op("act", lambda e: e.activation(out=junk_ap, in_=xs_ap, func=AF.Square, accum_out=ss),
             reads=[xs_key], writes=[junk_key, kss])
        S.op("dve", lambda e: e.tensor_scalar(out=vv, in0=ss, scalar1=1.0 / D, scalar2=EPS, op0=ALU.mult, op1=ALU.add),
             reads=[kss], writes=[kvv])
        S.op("act", lambda e: e.activation(out=vv, in_=vv, func=AF.Sqrt), reads=[kvv], writes=[kvv])
        S.op("dve", lambda e: e.reciprocal(out=rs, in_=vv), reads=[kvv], writes=[krs])
        return rs, krs

    def load_x(layer, s, from_input=False):
        key, ap, sem = xs_ring.next()
        src = xin_v[:, s, :] if from_input else xres_v[:, s, :]
        rd = [] if from_input else [("xres", s)]
        S.dma("sp", ap, src, "ld_" + sem, reads=rd, writes=[key])
        return key, ap

    def store_x(ap, key, s):
        S.dma("sp", xres_v[:, s, :], ap, "st_" + key[0] + str(key[1]), reads=[key], writes=[("xres", s)])

    def compute_mod(l, half, wst_ring, norm_row):
        S.dma("sp", gbc, d_ng[norm_row:norm_row + 1, :].partition_broadcast(128), "gbc", writes=["gbc"])
        for nb in range(6):
            col0 = half * 3072 + nb * 512
            wk, wap, wsem = wst_ring.next()
            S.dma("pool", wap, d_w_ada[l, :, col0:col0 + 512].rearrange("(k p) n -> p k n", p=128), "w_" + wsem,
                  writes=[wk])
            bk, bap, bsem = bb_ring.next()
            S.dma("sp", bap, d_b_ada[l:l + 1, col0:col0 + 512].partition_broadcast(128), "b_" + bsem, writes=[bk])
            bank = rot_z.next()

            def mm(e, wap=wap, bank=bank):
                for k in range(8):
                    ins = e.matmul(ps[bank][:], lhsT=cactT[:, k, :], rhs=wap[:, k, :], start=(k == 0), stop=(k == 7))
                return ins
            S.op("pe", mm, reads=[wk, "cactT"], writes=[PSK(bank)])
            j, h2 = nb // 2, nb % 2
            dst = mod3[:, j, h2 * 512:(h2 + 1) * 512]
            S.op("dve", lambda e, dst=dst, bank=bank, bap=bap: e.tensor_tensor(out=dst, in0=ps[bank][:], in1=bap, op=ALU.add),
                 reads=[PSK(bank), bk], writes=[("mod3", j, h2)])
        S.op("dve", lambda e: e.scalar_tensor_tensor(out=mod3[:, 1, :], in0=mod3[:, 1, :], scalar=1.0, in1=gbc,
                                                     op0=ALU.add, op1=ALU.mult),
             reads=[("mod3", 1, 0), ("mod3", 1, 1), "gbc"], writes=[("mod3", 1, 0), ("mod3", 1, 1)])

    MOD_ALL = [("mod3", j, h) for j in range(3) for h in range(2)]

    def norm_front(xs_ap, xs_key):
        hk, hap, _ = htm_ring.next()
        rs, krs = rmsnorm_rstd(xs_ap, xs_key, hap, hk)
        return (xs_ap, xs_key, hk, hap, rs, krs)

    def norm_back(fr):
        xs_ap, xs_key, hk, hap, rs, krs = fr
        S.op("dve", lambda e: e.scalar_tensor_tensor(out=hap, in0=xs_ap, scalar=rs, in1=mod3[:, 1, :], op0=ALU.mult, op1=ALU.mult),
             reads=[xs_key, krs, ("mod3", 1, 0), ("mod3", 1, 1)], writes=[hk])
        S.op("dve", lambda e: e.tensor_tensor(out=hap, in0=hap, in1=mod3[:, 0, :], op=ALU.add),
             reads=[hk, ("mod3", 0, 0), ("mod3", 0, 1)], writes=[hk])
        return hk, hap

    def transpose_to_fm(hk, hap, dst_fn, dst_keys, extra32=None):
        for half in range(2):
            bank = rot_tr.next()

            def tr(e, half=half, bank=bank):
                for q in range(4):
                    k = half * 4 + q
                    ins = e.transpose(ps[bank][:, q * 128:(q + 1) * 128], hap[:, k * 128:(k + 1) * 128], ident)
                return ins
            S.op("pe", tr, reads=[hk, "ident"], writes=[PSK(bank)])
            dst = dst_fn(half * 4)
            src = ps[bank][:].rearrange("p (a b) -> p a b", b=128)
            if extra32 is not None:
                d32, k32 = extra32(half * 4)
                S.op("act", lambda e, d32=d32, src=src: e.activation(out=d32, in_=src, func=AF.Copy),
                     reads=[PSK(bank)], writes=[k32])
                S.op("dve", lambda e, dst=dst, d32=d32: e.tensor_copy(out=dst, in_=d32),
                     reads=[k32], writes=[dst_keys[half]])
                continue
            eng = "act" if half == 0 else "dve"
            if eng == "act":
                S.op("act", lambda e, dst=dst, src=src: e.activation(out=dst, in_=src, func=AF.Copy),
                     reads=[PSK(bank)], writes=[dst_keys[half]])
            else:
                S.op("dve", lambda e, dst=dst, src=src: e.tensor_copy(out=dst, in_=src),
                     reads=[PSK(bank)], writes=[dst_keys[half]])

    def mixer_phase(l):
        mc = Carver(PH0, ARENA)
        wst = [mc.get(8 * 512 * 2, BF16, (8, 512)) for _ in range(3)]
        wst_ring = Ring("wst", wst)
        wrga = mc.get(8 * 128 * 2, BF16, (8, 128))
        wrgx = mc.get(8 * 128 * 2, BF16, (8, 128))
        wpool = mc.get(4 * 128 * 2, BF16, (4, 128))
        wupa = mc.get(8 * 1024 * 2, BF16, (8, 1024))
        wupb = mc.get(4 * 1024 * 2, BF16, (4, 1024))
        wout = mc.get(8 * 1024 * 2, BF16, (8, 1024))
        dg = mc.get(32 * 128 * 2, BF16, (32, 128))
        hfm = [mc.get(8 * 512 * 2, BF16, (8, 512)) for _ in range(2)]
        ylru = mc.get(8 * 512 * 2, BF16, (8, 512))
        ypool = mc.get(4 * 512 * 2, BF16, (4, 512))
        merged = mc.get(8 * 512 * 2, BF16, (8, 512))
        tmps = [mc.get(512 * 4, F32) for _ in range(16)]
        tmp_ring = Ring("tmp", tmps)
        xlb = [mc.get(516 * 2, BF16) for _ in range(2)]
        xlb_ring = Ring("xlb", xlb)
        xcb = [mc.get(512 * 2, BF16) for _ in range(4)]
        xcb_ring = Ring("xcb", xcb)
        xpb = [mc.get(528 * 4, F32) for _ in range(2)]
        xpb_ring = Ring("xpb", xpb)
        xpt = [mc.get(528 * 4, F32) for _ in range(2)]
        xpt_ring = Ring("xpt", xpt)
        plb = [mc.get(512 * 2, BF16) for _ in range(2)]
        plb_ring = Ring("plb", plb)

        gk = ("mw", l)
        S.dma("pool", wrga, d_w_rg_a[l].rearrange("h i o -> i h o"), "mw", writes=["wrga"], group=gk)
        S.dma("pool", wrgx, d_w_rg_x[l].rearrange("h i o -> i h o"), "mw", writes=["wrgx"], group=gk)
        S.dma("pool", wpool, d_w_pool[l].rearrange("h i o -> i h o"), "mw", writes=["wpool"], group=gk)
        S.dma("pool", wupa, d_w_up_a[l].rearrange("(k p) n -> p k n", p=128), "mw", writes=["wupa"], group=gk)
        S.dma("pool", wupb, d_w_up_b[l].rearrange("(k p) n -> p k n", p=128), "mw", writes=["wupb"], group=gk)
        S.dma("pool", wout, d_w_out[l].rearrange("(k p) n -> p k n", p=128), "mw", writes=["wout"], group=gk)

        compute_mod(l, 0, wst_ring, l)

        for c in range(8):
            for k in range(4):
                j = c * 4 + k
                col = cv(l, "cw", k * 8 + c)
                S.op("dve", lambda e, j=j, col=col: e.tensor_scalar(out=dg[:, j, :], in0=ident, scalar1=cvec[:, col:col + 1],
                                                                   scalar2=None, op0=ALU.mult),
                     reads=["ident", "cvec"], writes=[("dg", j)])
        lam = cvec[:, cv(l, "lam"):cv(l, "lam") + 8]
        t8 = stat[:, 60:68] if False else small[:, 104:112]
        S.op("act", lambda e: e.activation(out=t8, in_=lam, func=AF.Exp, scale=-1.0), reads=["cvec"], writes=["t8"])
        S.op("act", lambda e: e.activation(out=t8, in_=t8, func=AF.Ln, bias=1.0), reads=["t8"], writes=["t8"])
        S.op("dve", lambda e: e.tensor_scalar(out=m8sp, in0=t8, scalar1=-8.0, scalar2=None, op0=ALU.mult), reads=["t8"], writes=["m8sp"])
        S.op("dve", lambda e: e.memset(hcar, 0.0), writes=[("hcar", c) for c in range(8)])
        S.op("dve", lambda e: e.memset(ccar, 0.0), writes=[("ccar", c) for c in range(8)])
        S.op("dve", lambda e: e.memset(pcar, 0.0), writes=[("pcar", g) for g in range(4)])

        n_tiles = S_LEN // 512
        for ti in range(n_tiles):
            hf = hfm[ti % 2]
            hfk = ("hfm", ti % 2)
            def m_front(j, ti=ti):
                xk, xap = load_x(l, ti * 4 + j, from_input=(l == 0))
                return norm_front(xap, xk)
            fr = m_front(0)
            for j in range(4):
                fr_next = m_front(j + 1) if j + 1 < 4 else None
                hk, hap = norm_back(fr)
                fr = fr_next
                transpose_to_fm(hk, hap, lambda k0, j=j, hf=hf: hf[:, k0:k0 + 4, j * 128:(j + 1) * 128],
                                [hfk + (j, 0), hfk + (j, 1)])
            hf_keys = [hfk + (j, h) for j in range(4) for h in range(2)]

            def inproj(wap, wk, q, hf=hf, hf_keys=hf_keys):
                bank = rot_z.next()

                def mm(e, wap=wap, q=q, bank=bank):
                    for k in range(8):
                        ins = e.matmul(ps[bank][:], lhsT=wap[:, k, q * 128:(q + 1) * 128], rhs=hf[:, k, :],
                                       start=(k == 0), stop=(k == 7))
                    return ins
                S.op("pe", mm, reads=[wk] + hf_keys, writes=[PSK(bank)])
                return bank

            def load_piece(col0, ncols=512, part=None, ring_slot=None):
                if ring_slot is None:
                    wk, wap, wsem = wst_ring.next()
                else:
                    wk, wap, wsem = ring_slot
                if part is None:
                    S.dma("pool", wap, d_w_in[l, :, col0:col0 + ncols].rearrange("(k p) n -> p k n", p=128), "w_" + wsem,
                          writes=[wk])
                return wk, wap, wsem

            gq = []
            for pc_ in range(2):
                wk, wap, _ = load_piece(1024 + pc_ * 512)
                for q in range(4):
                    gq.append((pc_ * 4 + q, wap, wk, q))

            def g_stage1(c, wap, wk, q):
                bank = inproj(wap, wk, q)
                gk_, g_, _ = tmp_ring.next()
                tk_, t_, _ = tmp_ring.next()
                S.op("act", lambda e, g_=g_, bank=bank: e.activation(out=g_, in_=ps[bank][:], func=AF.Copy),
                     reads=[PSK(bank)], writes=[gk_])
                S.op("dve", lambda e, g_=g_, t_=t_: e.scalar_tensor_tensor(out=t_, in0=g_, scalar=0.044715, in1=g_,
                                                                          op0=ALU.mult, op1=ALU.mult),
                     reads=[gk_], writes=[tk_])
                S.op("dve", lambda e, g_=g_, t_=t_: e.scalar_tensor_tensor(out=t_, in0=t_, scalar=1.0, in1=g_,
                                                                          op0=ALU.add, op1=ALU.mult),
                     reads=[gk_, tk_], writes=[tk_])
                return (c, gk_, g_, tk_, t_)

            def g_stage2(c, gk_, g_, tk_, t_):
                S.op("act", lambda e, t_=t_: e.activation(out=t_, in_=t_, func=AF.Sigmoid, scale=1.5957691216057308),
                     reads=[tk_], writes=[tk_])
                S.op("dve", lambda e, g_=g_, t_=t_, c=c: e.tensor_tensor(out=ylru[:, c, :], in0=g_, in1=t_, op=ALU.mult),
                     reads=[gk_, tk_], writes=[("ylru", c)])

            pend = g_stage1(*gq[0])
            for n in range(8):
                nxt = g_stage1(*gq[n + 1]) if n + 1 < 8 else None
                g_stage2(*pend)
                pend = nxt

            for pc_ in range(2):
                wk, wap, _ = load_piece(pc_ * 512)
                st8 = [dict() for _ in range(4)]

                def l_stage1(q, wap=wap, wk=wk, pc_=pc_, st8=st8):
                    c = pc_ * 4 + q
                    bank = inproj(wap, wk, q)
                    xk_, xl_, _ = xlb_ring.next()
                    S.op("act", lambda e, xl_=xl_, c=c: e.activation(out=xl_[:, 0:3], in_=ccar[:, c, 0:3], func=AF.Copy),
                         reads=[("ccar", c)], writes=[xk_])
                    S.op("act", lambda e, xl_=xl_, bank=bank: e.activation(out=xl_[:, 3:515], in_=ps[bank][:], func=AF.Copy),
                         reads=[PSK(bank)], writes=[xk_])
                    S.op("act", lambda e, xl_=xl_, c=c: e.activation(out=ccar[:, c, 0:3], in_=xl_[:, 512:515], func=AF.Copy),
                         reads=[xk_], writes=[("ccar", c)])
                    st8[q].update(c=c, xk=xk_, xl=xl_)

                def l_stage2(q, st8=st8):
                    d_ = st8[q]
                    c, xk_, xl_ = d_["c"], d_["xk"], d_["xl"]
                    b2 = rot_aux.next()

                    def conv(e, xl_=xl_, c=c, b2=b2):
                        for k in range(4):
                            ins = e.matmul(ps[b2][:], lhsT=dg[:, c * 4 + k, :], rhs=xl_[:, k:k + 512], start=(k == 0), stop=(k == 3))
                        return ins
                    S.op("pe", conv, reads=[xk_] + [("dg", c * 4 + k) for k in range(4)], writes=[PSK(b2)])
                    xck, xc_, _ = xcb_ring.next()
                    cb_col = cv(l, "cb", c)
                    S.op("act", lambda e, xc_=xc_, b2=b2, cb_col=cb_col: e.activation(out=xc_, in_=ps[b2][:], func=AF.Identity,
                                                                                     bias=cvec[:, cb_col:cb_col + 1]),
                         reads=[PSK(b2), "cvec"], writes=[xck])
                    d_.update(xck=xck, xc=xc_)

                def l_stage3(q, st8=st8):
                    d_ = st8[q]
                    c, xck, xc_ = d_["c"], d_["xck"], d_["xc"]
                    b3 = rot_aux.next()
                    S.op("pe", lambda e, b3=b3, c=c, xc_=xc_: e.matmul(ps[b3][:], lhsT=wrga[:, c, :], rhs=xc_, start=True, stop=True),
                         reads=[xck, "wrga"], writes=[PSK(b3)])
                    b4 = rot_aux.next()
                    S.op("pe", lambda e, b4=b4, c=c, xc_=xc_: e.matmul(ps[b4][:], lhsT=wrgx[:, c, :], rhs=xc_, start=True, stop=True),
                         reads=[xck, "wrgx"], writes=[PSK(b4)])
                    rk, r_, _ = tmp_ring.next()
                    ik, i_, _ = tmp_ring.next()
                    mk, m_, _ = tmp_ring.next()
                    ba_col = cv(l, "ba", c)
                    bx_col = cv(l, "bx", c)
                    S.op("act", lambda e, r_=r_, b3=b3, ba_col=ba_col: e.activation(out=r_, in_=ps[b3][:], func=AF.Sigmoid,
                                                                                   bias=cvec[:, ba_col:ba_col + 1]),
                         reads=[PSK(b3), "cvec"], writes=[rk])
                    S.op("act", lambda e, i_=i_, b4=b4, bx_col=bx_col: e.activation(out=i_, in_=ps[b4][:], func=AF.Sigmoid,
                                                                                   bias=cvec[:, bx_col:bx_col + 1]),
                         reads=[PSK(b4), "cvec"], writes=[ik])
                    d_.update(rk=rk, r=r_, ik=ik, i=i_, mk=mk, m=m_)

                for step in range(6):
                    if step < 4:
                        l_stage1(step)
                    if 1 <= step <= 4:
                        l_stage2(step - 1)
                    if 2 <= step <= 5:
                        l_stage3(step - 2)
                for q in range(4):
                    d_ = st8[q]
                    S.op("act", lambda e, r_=d_["r"], c=d_["c"]: e.activation(out=r_, in_=r_, func=AF.Exp, scale=m8sp[:, c:c + 1]),
                         reads=[d_["rk"], "m8sp"], writes=[d_["rk"]])
                for q in range(4):
                    d_ = st8[q]
                    S.op("dve", lambda e, a_=d_["r"], m_=d_["m"]: e.scalar_tensor_tensor(out=m_, in0=a_, scalar=-1.0, in1=a_,
                                                                                      op0=ALU.mult, op1=ALU.mult),
                         reads=[d_["rk"]], writes=[d_["mk"]])
                    S.op("dve", lambda e, i_=d_["i"], xc_=d_["xc"]: e.tensor_tensor(out=i_, in0=i_, in1=xc_, op=ALU.mult),
                         reads=[d_["ik"], d_["xck"]], writes=[d_["ik"]])
                for q in range(4):
                    d_ = st8[q]
                    S.op("act", lambda e, m_=d_["m"]: e.activation(out=m_, in_=m_, func=AF.Sqrt, bias=1.0, scale=1.0),
                         reads=[d_["mk"]], writes=[d_["mk"]])
                for q in range(4):
                    d_ = st8[q]
                    c = d_["c"]
                    r_, i_, m_ = d_["r"], d_["i"], d_["m"]
                    rk, ik, mk = d_["rk"], d_["ik"], d_["mk"]
                    S.op("dve", lambda e, i_=i_, m_=m_: e.tensor_tensor(out=i_, in0=i_, in1=m_, op=ALU.mult),
                         reads=[ik, mk], writes=[ik])
                    S.op("dve", lambda e, r_=r_, m_=m_, i_=i_, c=c: e.tensor_tensor_scan(out=m_, data0=r_, data1=i_,
                                                                                        initial=hcar[:, c:c + 1],
                                                                                        op0=ALU.mult, op1=ALU.add),
                         reads=[rk, ik, ("hcar", c), mk], writes=[mk])
                    S.op("act", lambda e, m_=m_, c=c: e.activation(out=hcar[:, c:c + 1], in_=m_[:, 511:512], func=AF.Copy),
                         reads=[mk], writes=[("hcar", c)])
                    S.op("dve", lambda e, m_=m_, c=c: e.tensor_tensor(out=ylru[:, c, :], in0=ylru[:, c, :], in1=m_, op=ALU.mult),
                         reads=[mk, ("ylru", c)], writes=[("ylru", c)])

            wk, wap, _ = load_piece(2048)
            for g in range(4):
                w = 2 << g
                bank = inproj(wap, wk, g)
                pk, pb, _ = xpb_ring.next()
                S.op("act", lambda e, pb=pb, g=g: e.activation(out=pb[:, 0:15], in_=pcar[:, g, 0:15], func=AF.Copy),
                     reads=[("pcar", g)], writes=[pk])
                S.op("act", lambda e, pb=pb, bank=bank: e.activation(out=pb[:, 15:527], in_=ps[bank][:], func=AF.Copy),
                     reads=[PSK(bank)], writes=[pk])
                S.op("act", lambda e, pb=pb, g=g: e.activation(out=pcar[:, g, 0:15], in_=pb[:, 512:527], func=AF.Copy),
                     reads=[pk], writes=[("pcar", g)])
                cur, curk, lo = pb, pk, 0
                sh = 1
                for step in range(g + 1):
                    nk, nb_, _ = xpt_ring.next()
                    n = 527 - (lo + sh)
                    S.op("dve", lambda e, nb_=nb_, cur=cur, lo=lo, sh=sh, n=n: e.tensor_tensor(
                        out=nb_[:, lo + sh:527], in0=cur[:, lo + sh:527], in1=cur[:, lo:lo + n], op=ALU.add),
                        reads=[curk], writes=[nk])
                    cur, curk, lo = nb_, nk, lo + sh
                    sh *= 2
                plk, pl_, _ = plb_ring.next()
                S.op("dve", lambda e, pl_=pl_, cur=cur, pb=pb, w=w: e.scalar_tensor_tensor(
                    out=pl_, in0=cur[:, 15:527], scalar=1.0 / w, in1=pb[:, 15:527], op0=ALU.mult, op1=ALU.subtract),
                    reads=[curk, pk], writes=[plk])
                if ti == 0:
                    tk_, t_, _ = tmp_ring.next()
                    S.op("dve", lambda e, t_=t_, cur=cur, g=g: e.tensor_tensor(out=t_[:, 0:16], in0=cur[:, 15:31], in1=cinv[:, g, :], op=ALU.mult),
                         reads=[curk, ("cinv", g)], writes=[tk_])
                    S.op("dve", lambda e, t_=t_, pl_=pl_, pb=pb: e.tensor_tensor(out=pl_[:, 0:16], in0=t_[:, 0:16], in1=pb[:, 15:31], op=ALU.subtract),
                         reads=[tk_, pk, plk], writes=[plk])
                b2 = rot_aux.next()
                S.op("pe", lambda e, b2=b2, g=g, pl_=pl_: e.matmul(ps[b2][:], lhsT=wpool[:, g, :], rhs=pl_, start=True, stop=True),
                     reads=[plk, "wpool"], writes=[PSK(b2)])
                psc = cv(l, "ps", g)
                S.op("act", lambda e, g=g, b2=b2, psc=psc: e.activation(out=ypool[:, g, :], in_=ps[b2][:], func=AF.Copy,
                                                                         scale=cvec[:, psc:psc + 1]),
                     reads=[PSK(b2), "cvec"], writes=[("ypool", g)])

            for qq in range(4):
                wk, wap, wsem = wst_ring.next()
                cA = 2560 + qq * 256
                cB = 2560 + 1024 + qq * 256
                S.dma("pool", wap[:, :, 0:256], d_w_in[l, :, cA:cA + 256].rearrange("(k p) n -> p k n", p=128), "w_" + wsem,
                      writes=[wk])
                S.dma("pool", wap[:, :, 256:512], d_w_in[l, :, cB:cB + 256].rearrange("(k p) n -> p k n", p=128), "w_" + wsem,
                      writes=[wk])
                gts = []
                for q in range(4):
                    bank = rot_z.next()

                    def mm(e, wap=wap, q=q, bank=bank, hf=hf):
                        for k in range(8):
                            ins = e.matmul(ps[bank][:], lhsT=wap[:, k, q * 128:(q + 1) * 128], rhs=hf[:, k, :],
                                           start=(k == 0), stop=(k == 7))
                        return ins
                    S.op("pe", mm, reads=[wk] + hf_keys, writes=[PSK(bank)])
                    gk_, g_, _ = tmp_ring.next()
                    S.op("act", lambda e, g_=g_, bank=bank: e.activation(out=g_, in_=ps[bank][:], func=AF.Sigmoid),
                         reads=[PSK(bank)], writes=[gk_])
                    gts.append((gk_, g_))
                for cc in range(2):
                    c = qq * 2 + cc
                    g0k, g0 = gts[cc]
                    g1k, g1 = gts[2 + cc]
                    ba_ = rot_aux.next()

                    def mma(e, c=c, ba_=ba_):
                        for k in range(8):
                            ins = e.matmul(ps[ba_][:], lhsT=wupa[:, k, c * 128:(c + 1) * 128], rhs=ylru[:, k, :],
                                           start=(k == 0), stop=(k == 7))
                        return ins
                    S.op("pe", mma, reads=["wupa"] + [("ylru", k) for k in range(8)], writes=[PSK(ba_)])
                    bb_ = rot_aux.next()

                    def mmb(e, c=c, bb_=bb_):
                        for k in range(4):
                            ins = e.matmul(ps[bb_][:], lhsT=wupb[:, k, c * 128:(c + 1) * 128], rhs=ypool[:, k, :],
                                           start=(k == 0), stop=(k == 3))
                        return ins
                    S.op("pe", mmb, reads=["wupb"] + [("ypool", k) for k in range(4)], writes=[PSK(bb_)])
                    S.op("dve", lambda e, g0=g0, ba_=ba_: e.tensor_tensor(out=g0, in0=g0, in1=ps[ba_][:], op=ALU.mult),
                         reads=[g0k, PSK(ba_)], writes=[g0k])
                    S.op("dve", lambda e, g1=g1, bb_=bb_: e.tensor_tensor(out=g1, in0=g1, in1=ps[bb_][:], op=ALU.mult),
                         reads=[g1k, PSK(bb_)], writes=[g1k])
                    S.op("dve", lambda e, g0=g0, g1=g1, c=c: e.tensor_tensor(out=merged[:, c, :], in0=g0, in1=g1, op=ALU.add),
                         reads=[g0k, g1k], writes=[("merged", c)])

            for j in range(4):
                s = ti * 4 + j
                xk, xap = load_x(l, s, from_input=(l == 0))
                for hh in range(2):
                    bank = rot_z.next()

                    def mmo(e, j=j, hh=hh, bank=bank):
                        for k in range(8):
                            ins = e.matmul(ps[bank][:], lhsT=merged[:, k, j * 128:(j + 1) * 128],
                                           rhs=wout[:, k, hh * 512:(hh + 1) * 512], start=(k == 0), stop=(k == 7))
                        return ins
                    S.op("pe", mmo, reads=["wout"] + [("merged", k) for k in range(8)], writes=[PSK(bank)])
                    tk_, t_, _ = tmp_ring.next()
                    S.op("dve", lambda e, t_=t_, bank=bank, hh=hh: e.tensor_tensor(out=t_, in0=ps[bank][:],
                                                                                  in1=mod3[:, 2, hh * 512:(hh + 1) * 512], op=ALU.mult),
                         reads=[PSK(bank), ("mod3", 2, hh)], writes=[tk_])
                    S.op("dve", lambda e, t_=t_, xap=xap, hh=hh: e.tensor_tensor(out=xap[:, hh * 512:(hh + 1) * 512],
                                                                                in0=xap[:, hh * 512:(hh + 1) * 512], in1=t_, op=ALU.add),
                         reads=[tk_, xk], writes=[xk])
                store_x(xap, xk, s)

    def moe_phase(l, last):
        mc = Carver(PH0, ARENA)
        wex = [mc.get(8 * 1024 * 2, BF16, (8, 1024)) for _ in range(5)]
        wex_ring = Ring("wex", wex)
        wst_ring = Ring("wex", [w_[:, :, 0:512] for w_ in wex])
        hfm = mc.get(8 * 1024 * 2, BF16, (8, 1024))
        yacc = mc.get(8 * 1024 * 4, F32, (8, 1024))
        actb = [mc.get(8 * 512 * 2, BF16, (8, 512)) for _ in range(2)]
        hT32 = mc.get(8 * 128 * 4, F32, (8, 128))
        gates = mc.get(8 * 32 * 4, F32, (8, 32))
        lgb = [mc.get(32 * 4, F32) for _ in range(2)]
        lg_ring = Ring("lg", lgb)
        emb = [mc.get(32 * 4, F32) for _ in range(2)]
        em_ring = Ring("em", emb)
        mx8 = [mc.get(8 * 4, F32) for _ in range(2)]
        mx_ring = Ring("mx", mx8)
        gpad = mc.get(128 * 4, F32)
        gT = [mc.get(128 * 4, F32) for _ in range(2)]
        gT_ring = Ring("gT", gT)
        beo = gbc
        tb0 = mc.off
        tmps = [mc.get(512 * 4, F32) for _ in range(6)]
        tmp_ring = Ring("tmp", tmps)
        tbig = arena[:, tb0:tb0 + 4096].bitcast(F32)
        TBK = [("tmp", 0), ("tmp", 1)]

        compute_mod(l, 1, wst_ring, 4 + l)
        S.op("dve", lambda e: e.memset(gpad, 0.0), writes=["gpad"])
        gk = ("moew", l)
        S.dma("sp", wr32, d_w_router[l].rearrange("(k p) n -> p k n", p=128), "mwc", writes=["wr32"], group=gk)
        S.dma("sp", brbc, d_b_router[l:l + 1, :].partition_broadcast(128), "mwc", writes=["brbc"], group=gk)
        S.dma("sp", b1, d_b1[:, l * 512:(l + 1) * 512].rearrange("p (a b) -> p a b", b=16), "mwc", writes=["b1"], group=gk)
        S.dma("sp", beo[0:32, :], d_b_e_out[l], "gbc", writes=["gbc"])
        S.op("dve", lambda e: e.tensor_scalar(out=b1[:, :, 8:16], in0=b1[:, :, 8:16], scalar1=1.0, scalar2=None, op0=ALU.add),
             reads=["b1"], writes=["b1"])

        rot_gu = Rot([2, 3, 4])
        rot_o = Rot([5, 6])
        MISC = 7

        n_pass = dbg_np
        for p in range(n_pass):
            def e_front(j, p=p):
                xk, xap = load_x(l, p * 8 + j, from_input=dbg_skip_mixer)
                return norm_front(xap, xk)
            fr = e_front(0)
            for j in range(8 if dbg_cut > 0 else 0):
                s = p * 8 + j
                fr_next = e_front(j + 1) if j + 1 < 8 else None
                hk, hap = norm_back(fr)
                fr = fr_next
                transpose_to_fm(hk, hap, lambda k0, j=j: hfm[:, k0:k0 + 4, j * 128:(j + 1) * 128],
                                [("hfm", j, 0), ("hfm", j, 1)],
                                extra32=(None if (dbg_f & 1) else (lambda k0: (hT32[:, k0:k0 + 4, :], ("hT32", k0 // 4)))))

                if dbg_cut <= 1:
                    continue

                def rmm(e):
                    for k in range(8):
                        ins = e.matmul(ps[MISC][:, 0:32], lhsT=hT32[:, k, :], rhs=wr32[:, k, :], start=(k == 0), stop=(k == 7))
                    return ins
                S.op("pe", rmm, reads=[("hT32", 0), ("hT32", 1), "wr32"], writes=[PSK(MISC)])
                lk, lg, _ = lg_ring.next()
                ek, em, _ = em_ring.next()
                mk_, mx, _ = mx_ring.next()
                S.op("dve", lambda e, lg=lg: e.tensor_tensor(out=lg, in0=ps[MISC][:, 0:32], in1=brbc, op=ALU.add),
                     reads=[PSK(MISC), "brbc"], writes=[lk])
                if dbg_cut <= 2:
                    continue
                S.op("dve", lambda e, lg=lg, mx=mx: e.max(out=mx, in_=lg), reads=[lk], writes=[mk_])
                if dbg_cut <= 3:
                    continue
                c0 = stat_col(3)
                negm = stat[:, c0:c0 + 1]
                den = stat[:, c0 + 1:c0 + 2]
                rden = stat[:, c0 + 2:c0 + 3]
                S.op("dve", lambda e, mx=mx, negm=negm: e.tensor_scalar(out=negm, in0=mx[:, 0:1], scalar1=-1.0, scalar2=None, op0=ALU.mult),
                     reads=[mk_], writes=[("stat", c0)])
                S.op("act", lambda e, em=em, lg=lg, negm=negm: e.activation(out=em, in_=lg, func=AF.Exp, bias=negm, scale=1.0),
                     reads=[lk, ("stat", c0)], writes=[ek])
                S.op("dve", lambda e, lg=lg, mx=mx: e.tensor_scalar(out=lg, in0=lg, scalar1=mx[:, 3:4], scalar2=None, op0=ALU.is_ge),
                     reads=[lk, mk_, ek], writes=[lk])
                S.op("dve", lambda e, em=em, lg=lg, den=den: e.scalar_tensor_tensor(out=em, in0=em, scalar=1.0, in1=lg,
                                                                                   op0=ALU.mult, op1=ALU.mult, accum_out=den),
                     reads=[ek, lk], writes=[ek, ("stat", c0 + 1)])
                S.op("dve", lambda e, den=den, rden=rden: e.reciprocal(out=rden, in_=den), reads=[("stat", c0 + 1)], writes=[("stat", c0 + 2)])
                S.op("dve", lambda e, em=em, rden=rden, j=j: e.tensor_scalar(out=gates[:, j, :], in0=em, scalar1=rden, scalar2=None, op0=ALU.mult),
                     reads=[ek, ("stat", c0 + 2)], writes=[("gates", j)])
                if dbg_cut <= 4:
                    continue
                S.op("dve", lambda e, j=j: e.tensor_copy(out=gpad[:, 0:32], in_=gates[:, j, :]),
                     reads=[("gates", j)], writes=["gpad"])
                S.op("pe", lambda e: e.transpose(ps[MISC][:, 128:256], gpad, ident),
                     reads=["gpad", "ident"], writes=[PSK(MISC)])
                gtk, gt, _ = gT_ring.next()
                S.op("act", lambda e, gt=gt: e.activation(out=gt[0:32, :], in_=ps[MISC][0:32, 128:256], func=AF.Copy),
                     reads=[PSK(MISC)], writes=[gtk])
                if dbg_cut <= 5:
                    continue
                for hh in range(2):
                    bo = rot_o.next()
                    S.op("pe", lambda e, gt=gt, hh=hh, bo=bo: e.matmul(ps[bo][:], lhsT=gt[0:32, :], rhs=beo[0:32, hh * 512:(hh + 1) * 512],
                                                                      start=True, stop=True),
                         reads=[gtk, "gbc"], writes=[PSK(bo)])
                    S.op("act", lambda e, j=j, hh=hh, bo=bo: e.activation(out=yacc[:, j, hh * 512:(hh + 1) * 512], in_=ps[bo][:], func=AF.Copy),
                         reads=[PSK(bo)], writes=[("yacc", j, hh)])

            hf_keys = [[("hfm", g * 4 + j4, h) for j4 in range(4) for h in range(2)] for g in range(2)]
            for ex in range(dbg_ne):
                wgk, wg, wgs = wex_ring.next()
                S.dma("pool", wg, d_w_e_in[l, ex, :, 0:1024].rearrange("(k p) n -> p k n", p=128), "e_" + wgs, writes=[wgk])
                wuk, wu, wus = wex_ring.next()
                S.dma("pool", wu, d_w_e_in[l, ex, :, 1024:2048].rearrange("(k p) n -> p k n", p=128), "e_" + wus, writes=[wuk])
                wok, wo, wos = wex_ring.next()
                S.dma("pool", wo, d_w_e_out[l, ex].rearrange("(k p) n -> p k n", p=128), "e_" + wos, writes=[wok])
                for g in range(2):
                    ab = actb[g]
                    for jp in range(8):
                        bg = rot_gu.next()

                        def mg(e, wg=wg, jp=jp, bg=bg, g=g):
                            for k in range(8):
                                ins = e.matmul(ps[bg][:], lhsT=wg[:, k, jp * 128:(jp + 1) * 128], rhs=hfm[:, k, g * 512:(g + 1) * 512],
                                               start=(k == 0), stop=(k == 7))
                            return ins
                        S.op("pe", mg, reads=[wgk] + hf_keys[g], writes=[PSK(bg)])
                        bu = rot_gu.next()

                        def mu(e, wu=wu, jp=jp, bu=bu, g=g):
                            for k in range(8):
                                ins = e.matmul(ps[bu][:], lhsT=wu[:, k, jp * 128:(jp + 1) * 128], rhs=hfm[:, k, g * 512:(g + 1) * 512],
                                               start=(k == 0), stop=(k == 7))
                            return ins
                        S.op("pe", mu, reads=[wuk] + hf_keys[g], writes=[PSK(bu)])
                        gck, gc, _ = tmp_ring.next()
                        sgk, sg, _ = tmp_ring.next()
                        uck, uc, _ = tmp_ring.next()
                        S.op("dve", lambda e, gc=gc, bg=bg, ex=ex, jp=jp: e.tensor_scalar(out=gc, in0=ps[bg][:], scalar1=b1[:, ex, jp:jp + 1],
                                                                                         scalar2=7.0, op0=ALU.add, op1=ALU.min),
                             reads=[PSK(bg), "b1"], writes=[gck])
                        S.op("act", lambda e, sg=sg, gc=gc: e.activation(out=sg, in_=gc, func=AF.Sigmoid, scale=1.702),
                             reads=[gck], writes=[sgk])
                        S.op("dve", lambda e, uc=uc, bu=bu, ex=ex, jp=jp: e.tensor_scalar(out=uc, in0=ps[bu][:], scalar1=b1[:, ex, 8 + jp:9 + jp],
                                                                                         scalar2=8.0, op0=ALU.add, op1=ALU.min),
                             reads=[PSK(bu), "b1"], writes=[uck])
                        S.op("dve", lambda e, gc=gc, sg=sg: e.tensor_tensor(out=gc, in0=gc, in1=sg, op=ALU.mult),
                             reads=[gck, sgk], writes=[gck])
                        S.op("dve", lambda e, ab=ab, jp=jp, uc=uc, gc=gc: e.scalar_tensor_tensor(out=ab[:, jp, :], in0=uc, scalar=-6.0, in1=gc,
                                                                                                op0=ALU.max, op1=ALU.mult),
                             reads=[uck, gck], writes=[("act", g, jp)])
                for g in range(2):
                    ab = actb[g]
                    for j4 in range(4):
                        js = g * 4 + j4
                        for hh in range(2):
                            bo = rot_o.next()

                            def mo(e, ab=ab, j4=j4, hh=hh, bo=bo, wo=wo):
                                for k in range(8):
                                    ins = e.matmul(ps[bo][:], lhsT=ab[:, k, j4 * 128:(j4 + 1) * 128], rhs=wo[:, k, hh * 512:(hh + 1) * 512],
                                                   start=(k == 0), stop=(k == 7))
                                return ins
                            S.op("pe", mo, reads=[wok] + [("act", g, k) for k in range(8)], writes=[PSK(bo)])
                            S.op("dve", lambda e, js=js, hh=hh, bo=bo, ex=ex: e.scalar_tensor_tensor(
                                out=yacc[:, js, hh * 512:(hh + 1) * 512], in0=ps[bo][:], scalar=gates[:, js, ex:ex + 1],
                                in1=yacc[:, js, hh * 512:(hh + 1) * 512], op0=ALU.mult, op1=ALU.add),
                                reads=[PSK(bo), ("gates", js), ("yacc", js, hh)], writes=[("yacc", js, hh)])
            for j in range(8 if (dbg_cut > 0 and not (dbg_f & 2)) else 0):
                s = p * 8 + j
                xk, xap = load_x(l, s, from_input=dbg_skip_mixer)
                S.op("dve", lambda e, j=j: e.tensor_tensor(out=tbig, in0=yacc[:, j, :], in1=mod3[:, 2, :], op=ALU.mult),
                     reads=[("yacc", j, 0), ("yacc", j, 1), ("mod3", 2, 0), ("mod3", 2, 1)], writes=TBK)
                S.op("dve", lambda e, xap=xap: e.tensor_tensor(out=xap, in0=xap, in1=tbig, op=ALU.add),
                     reads=TBK + [xk], writes=[xk])
                store_x(xap, xk, s)

    final_ops = []
    for l in range(n_layers):
        if not dbg_skip_mixer:
            mixer_phase(l)
            S.barrier()
        if stop_after == ("mixer", l):
            break
        moe_phase(l, last=(l == n_layers - 1 and stop_after is None))
        S.barrier()
        if stop_after == ("moe", l):
            break
    if stop_after is None:
        S.dma("sp", gbc, d_ng[8:9, :].partition_broadcast(128), "gbc", writes=["gbc"])
        for s in range(S_LEN // 128):
            xk, xap = load_x(1, s)
            hk, hap, _ = htm_ring.next()
            rs, krs = rmsnorm_rstd(xap, xk, hap, hk)
            S.op("dve", lambda e, xap=xap, rs=rs: e.scalar_tensor_tensor(out=xap, in0=xap, scalar=rs, in1=gbc, op0=ALU.mult, op1=ALU.mult),
                 reads=[xk, krs, "gbc"], writes=[xk])
            o = S.dma("sp", out_v[:, s, :], xap, "st_" + xk[0] + str(xk[1]), reads=[xk], writes=[("out", s)])
            final_ops.append(o)
    if not final_ops:
        for s in range(32):
            xk, xap = load_x(1, s)
            o = S.dma("sp", out_v[:, s, :], xap, "st_" + xk[0] + str(xk[1]), reads=[xk], writes=[("out", s)])
            final_ops.append(o)
    S.emit(final_wait_ops=final_ops)
    st.close()
    return nc


def make_in_maps(inp):
    f = lambda a: np.ascontiguousarray(np.asarray(a, dtype=np.float32))
    L = L_DEPTH
    cvec = np.zeros((128, L * NCV), np.float32)

    def fm(v):
        return np.asarray(v, np.float32).reshape(-1, 128).T

    for l in range(L):
        base = l * NCV
        for k in range(4):
            cvec[:, base + k * 8: base + k * 8 + 8] = fm(inp["conv_w"][l, k])
        cvec[:, base + 32: base + 40] = fm(inp["conv_b"][l])
        cvec[:, base + 40: base + 48] = fm(inp["b_rg_a"][l])
        cvec[:, base + 48: base + 56] = fm(inp["b_rg_x"][l])
        cvec[:, base + 56: base + 64] = fm(inp["lru_lambda"][l])
        cvec[:, base + 64: base + 68] = fm(inp["pool_scale"][l])
    b1 = np.zeros((128, L * 512), np.float32)
    be = np.asarray(inp["b_e_in"], np.float32)
    for l in range(L):
        b1[:, l * 512:(l + 1) * 512] = be[l].reshape(NE, 16, 128).transpose(2, 0, 1).reshape(128, 512)
    ng = np.concatenate([np.asarray(inp["norm1_g"], np.float32), np.asarray(inp["norm2_g"], np.float32),
                         np.asarray(inp["final_g"], np.float32)[None, :]], axis=0)
    ident = np.eye(128, dtype=np.float32)
    t = np.arange(512, dtype=np.float32)
    cinv = np.stack([1.0 / np.minimum(t[:16] + 1.0, float(w)) for w in (2, 4, 8, 16)]).astype(np.float32)
    shared = dict(
        w_ada=f(inp["w_ada"]), b_ada=f(inp["b_ada"]), w_in=f(inp["w_in"]), w_rg_a=f(inp["w_rg_a"]), w_rg_x=f(inp["w_rg_x"]),
        w_pool=f(inp["w_pool"]), w_up_a=f(inp["w_up_a"]), w_up_b=f(inp["w_up_b"]), w_out=f(inp["w_out"]),
        w_router=f(inp["w_router"]), b_router=f(inp["b_router"]), w_e_in=f(inp["w_e_in"]), b1=b1,
        w_e_out=f(inp["w_e_out"]), b_e_out=f(inp["b_e_out"]), ng=f(ng), cvec=cvec, ident=ident, cinv=cinv)
    x = f(inp["x"])
    c = np.asarray(inp["c"], np.float32)
    maps = []
    for b in range(x.shape[0]):
        m = dict(shared)
        m["x"] = x[b]
        m["cfm"] = np.ascontiguousarray(fm(c[b]))
        maps.append(m)
    return maps


_CACHE = {}


def kernel(**inputs):
    maps = make_in_maps(inputs)
    if "nc" not in _CACHE:
        _CACHE["nc"] = build_program()
    nc = _CACHE["nc"]
    res = run_bass_kernel_spmd(nc, maps, core_ids=list(range(len(maps))))
    out = np.stack([np.asarray(r["out"], dtype=np.float32) for r in res.results], axis=0)
    return out
```

```python
import contextlib
import numpy as np
import concourse.bass as bass
import concourse.mybir as mybir
from concourse.bass_utils import run_bass_kernel_spmd

F32 = mybir.dt.float32
BF16 = mybir.dt.bfloat16
U8 = mybir.dt.uint8
AF = mybir.ActivationFunctionType
ALU = mybir.AluOpType

ENGINES = ("pe", "act", "dve", "pool", "sp")
L_DEPTH = 4
D = 1024
S_LEN = 4096
NE = 32
EPS = 1e-6
NCV = 68


class Op:
    __slots__ = ("eng", "fn", "deps", "needs_inc", "ev_sem", "ev_val", "is_dma", "group")

    def __init__(self, eng, fn, is_dma=False):
        self.eng = eng
        self.fn = fn
        self.deps = []
        self.needs_inc = False
        self.ev_sem = None
        self.ev_val = None
        self.is_dma = is_dma
        self.group = None


class Sched:
    def __init__(self, nc):
        self.nc = nc
        self.ops = {e: [] for e in ENGINES}
        self.res_w = {}
        self.res_r = {}
        self.sem_names = []
        self.dma_counts = {}
        self.groups = {}
        self.last_dma = {}
        self.pending = {e: [] for e in ENGINES}

    def _add_deps(self, o, reads, writes):
        deps = []
        for r in reads:
            w = self.res_w.get(r)
            if w is not None:
                deps.append((w, "raw"))
        for w_ in writes:
            w = self.res_w.get(w_)
            if w is not None:
                deps.append((w, "waw"))
            rr = self.res_r.get(w_)
            if rr:
                for rd in rr.values():
                    deps.append((rd, "war"))
        if self.pending[o.eng]:
            for d in self.pending[o.eng]:
                deps.append((d, "bar"))
            self.pending[o.eng] = []
        seen = set()
        for d, kind in deps:
            if d is o or id(d) in seen:
                continue
            if d.eng == o.eng and not d.is_dma and not o.is_dma:
                if kind != "raw" or o.eng == "pe":
                    continue
            seen.add(id(d))
            o.deps.append(d)
            d.needs_inc = True
        for r in reads:
            rr = self.res_r.setdefault(r, {})
            key = o.eng if not o.is_dma else ("dma", id(o))
            rr[key] = o
        for w_ in writes:
            self.res_w[w_] = o
            self.res_r[w_] = {}

    def op(self, eng, fn, reads=(), writes=()):
        o = Op(eng, fn)
        self._add_deps(o, reads, writes)
        self.ops[eng].append(o)
        return o

    def dma(self, eng, out, in_, sem, reads=(), writes=(), group=None, **kw):
        def fn(e):
            return e.dma_start(out=out, in_=in_, **kw)
        o = Op(eng, fn, is_dma=True)
        if sem not in self.dma_counts:
            self.dma_counts[sem] = 0
            self.sem_names.append(sem)
        self.dma_counts[sem] += 1
        o.ev_sem = sem
        o.ev_val = 16 * self.dma_counts[sem]
        o.group = group
        if group is not None:
            self.groups.setdefault(group, []).append(o)
        self._add_deps(o, reads, writes)
        self.ops[eng].append(o)
        self.last_dma[sem] = o
        return o

    def barrier(self):
        lasts = []
        for e in ENGINES:
            for o in reversed(self.ops[e]):
                if not o.is_dma:
                    lasts.append(o)
                    break
        dmas = list(self.last_dma.values())
        for e in ENGINES:
            self.pending[e] = [o for o in lasts if o.eng != e] + dmas
        self.res_w = {}
        self.res_r = {}

    def emit(self, final_wait_ops=()):
        nc = self.nc
        for d in final_wait_ops:
            d.needs_inc = True
        for e in ENGINES:
            cnt = 0
            for o in self.ops[e]:
                if o.is_dma:
                    continue
                if o.needs_inc:
                    cnt += 1
                    o.ev_sem = "eng_" + e
                    o.ev_val = cnt
        for g, lst in self.groups.items():
            tot = max(o.ev_val for o in lst)
            for o in lst:
                o.ev_val = tot
        sem_keys = ["eng_" + e for e in ENGINES] + list(self.sem_names)
        with contextlib.ExitStack() as st:
            sems = {}
            for k in sem_keys:
                sems[k] = st.enter_context(nc.semaphore(k))
            block = st.enter_context(nc.Block())

            def run(engname, eng):
                waited = {}
                for o in self.ops[engname]:
                    need = {}
                    for d in o.deps:
                        if waited.get(d.ev_sem, 0) >= d.ev_val:
                            continue
                        need[d.ev_sem] = max(need.get(d.ev_sem, 0), d.ev_val)
                    for sk, v in need.items():
                        eng.wait_ge(sems[sk], v)
                        waited[sk] = v
                    inst = o.fn(eng)
                    if o.is_dma:
                        inst.then_inc(sems[o.ev_sem], 16)
                    elif o.needs_inc:
                        inst.then_inc(sems[o.ev_sem], 1)
                if engname == "sp":
                    need = {}
                    for d in final_wait_ops:
                        need[d.ev_sem] = max(need.get(d.ev_sem, 0), d.ev_val)
                    for sk, v in need.items():
                        if waited.get(sk, 0) < v:
                            eng.wait_ge(sems[sk], v)

            @block.tensor
            def _(eng):
                run("pe", eng)

            @block.scalar
            def _(eng):
                run("act", eng)

            @block.vector
            def _(eng):
                run("dve", eng)

            @block.gpsimd
            def _(eng):
                run("pool", eng)

            @block.sync
            def _(eng):
                run("sp", eng)


class Ring:
    def __init__(self, name, aps):
        self.name = name
        self.aps = aps
        self.n = len(aps)
        self.i = 0

    def next(self):
        k = self.i % self.n
        self.i += 1
        return (self.name, k), self.aps[k], "%s%d" % (self.name, k)


def cv(l, name, idx=0):
    base = l * NCV
    off = {"cw": 0, "cb": 32, "ba": 40, "bx": 48, "lam": 56, "ps": 64}[name]
    return base + off + idx


def build_program(n_layers=L_DEPTH, stop_after=None, use_gelu_tanh=False, dbg_ne=NE, dbg_np=4, dbg_skip_mixer=False, dbg_cut=99, dbg_f=0):
    nc = bass.Bass("TRN2", target_bir_lowering=False)

    def din(name, shape, dt=F32):
        return nc.dram_tensor(name, list(shape), dt, kind="ExternalInput").ap()

    d_x = din("x", [S_LEN, D])
    d_cfm = din("cfm", [128, 8])
    d_w_ada = din("w_ada", [L_DEPTH, D, 6 * D])
    d_b_ada = din("b_ada", [L_DEPTH, 6 * D])
    d_w_in = din("w_in", [L_DEPTH, D, 4608])
    d_w_rg_a = din("w_rg_a", [L_DEPTH, 8, 128, 128])
    d_w_rg_x = din("w_rg_x", [L_DEPTH, 8, 128, 128])
    d_w_pool = din("w_pool", [L_DEPTH, 4, 128, 128])
    d_w_up_a = din("w_up_a", [L_DEPTH, D, D])
    d_w_up_b = din("w_up_b", [L_DEPTH, 512, D])
    d_w_out = din("w_out", [L_DEPTH, D, D])
    d_w_router = din("w_router", [L_DEPTH, D, NE])
    d_b_router = din("b_router", [L_DEPTH, NE])
    d_w_e_in = din("w_e_in", [L_DEPTH, NE, D, 2 * D])
    d_b1 = din("b1", [128, L_DEPTH * 512])
    d_w_e_out = din("w_e_out", [L_DEPTH, NE, D, D])
    d_b_e_out = din("b_e_out", [L_DEPTH, NE, D])
    d_ng = din("ng", [9, D])
    d_cvec = din("cvec", [128, L_DEPTH * NCV])
    d_ident = din("ident", [128, 128])
    d_cinv = din("cinv", [4, 16])
    d_xres = nc.dram_tensor("xres", [S_LEN, D], F32, kind="Internal").ap()
    d_out = nc.dram_tensor("out", [S_LEN, D], F32, kind="ExternalOutput").ap()

    xin_v = d_x.rearrange("(s p) d -> p s d", p=128)
    xres_v = d_xres.rearrange("(s p) d -> p s d", p=128)
    out_v = d_out.rearrange("(s p) d -> p s d", p=128)

    st = contextlib.ExitStack()
    ARENA = 207 * 1024
    arena = st.enter_context(nc.sbuf_tensor("arena", [128, ARENA], U8))
    ps = [st.enter_context(nc.psum_tensor("ps%d" % i, [128, 512], F32)) for i in range(8)]

    class Carver:
        def __init__(self, base, limit):
            self.off = base
            self.limit = limit

        def get(self, nbytes_free, dt, shape=None):
            nb = (nbytes_free + 31) // 32 * 32
            a = arena[:, self.off:self.off + nbytes_free].bitcast(dt)
            self.off += nb
            assert self.off <= self.limit, (self.off, self.limit)
            if shape is not None and len(shape) == 2:
                a = a.rearrange("p (a b) -> p a b", b=shape[1])
            if shape is not None and len(shape) == 3:
                a = a.rearrange("p (a b c) -> p a b c", b=shape[1], c=shape[2])
            return a

    PERS = 41 * 1024
    pc = Carver(0, PERS)
    ident = pc.get(128 * 4, F32)
    cvec = pc.get(L_DEPTH * NCV * 4, F32)
    cactT = pc.get(8 * 128 * 2, BF16, (8, 128))
    mod3 = pc.get(3 * 1024 * 4, F32, (3, 1024))
    gbc = pc.get(1024 * 4, F32)
    xs = [pc.get(1024 * 4, F32) for _ in range(2)]
    htm = [pc.get(1024 * 4, F32) for _ in range(2)]
    small = pc.get(256 * 4, F32)
    cfm = small[:, 0:8]
    csig = small[:, 8:16]
    cact = small[:, 16:24]
    m8sp = small[:, 24:32]
    hcar = small[:, 32:40]
    stat = small[:, 40:104]
    ccar = pc.get(8 * 4 * 2, BF16, (8, 4))
    pcar = pc.get(4 * 16 * 4, F32, (4, 16))
    wr32 = pc.get(8 * 32 * 4, F32, (8, 32))
    brbc = pc.get(32 * 4, F32)
    b1 = pc.get(512 * 4, F32, (32, 16))
    cinv = pc.get(4 * 16 * 4, F32, (4, 16))
    pers_end = pc.off

    S = Sched(nc)
    xs_ring = Ring("xs", xs)
    htm_ring = Ring("htm", htm)
    bb_ring = Ring("htm", [h[:, 0:512] for h in htm])
    stat_i = [0]

    def stat_col(n=1):
        k = stat_i[0]
        stat_i[0] = (k + n) % 60
        if stat_i[0] < n:
            k = 0
            stat_i[0] = n
        return k

    class Rot:
        def __init__(self, banks):
            self.banks = banks
            self.i = 0

        def next(self):
            b = self.banks[self.i % len(self.banks)]
            self.i += 1
            return b

    rot_tr = Rot([0, 1])
    rot_z = Rot([2, 3, 4])
    rot_aux = Rot([5, 6, 7])

    def PSK(b):
        return ("ps", b)

    S.dma("sp", ident, d_ident, "cst", writes=["ident"], group="cst")
    S.dma("sp", cvec, d_cvec, "cst", writes=["cvec"], group="cst")
    S.dma("sp", cfm, d_cfm, "cst", writes=["cfm"], group="cst")
    for g in range(4):
        S.dma("sp", cinv[:, g, :], d_cinv[g:g + 1, :].partition_broadcast(128), "cst", writes=[("cinv", g)], group="cst")
    S.op("act", lambda e: e.activation(out=csig, in_=cfm, func=AF.Sigmoid), reads=["cfm"], writes=["csig"])
    S.op("dve", lambda e: e.tensor_tensor(out=cact, in0=cfm, in1=csig, op=ALU.mult), reads=["cfm", "csig"], writes=["cact"])
    for k in range(8):
        S.op("dve", lambda e, k=k: e.tensor_copy(out=cactT[:, k, :], in_=cact[:, k:k + 1].to_broadcast([128, 128])),
             reads=["cact"], writes=["cactT"])

    PH0 = PERS

    def rmsnorm_rstd(xs_ap, xs_key, junk_ap, junk_key):
        c0 = stat_col(3)
        ss = stat[:, c0:c0 + 1]
        vv = stat[:, c0 + 1:c0 + 2]
        rs = stat[:, c0 + 2:c0 + 3]
        kss, kvv, krs = ("stat", c0), ("stat", c0 + 1), ("stat", c0 + 2)
        S.op("act", lambda e: e.activation(out=junk_ap, in_=xs_ap, func=AF.Square, accum_out=ss),
             reads=[xs_key], writes=[junk_key, kss])
        S.op("dve", lambda e: e.tensor_scalar(out=vv, in0=ss, scalar1=1.0 / D, scalar2=EPS, op0=ALU.mult, op1=ALU.add),
             reads=[kss], writes=[kvv])
        S.op("act", lambda e: e.activation(out=vv, in_=vv, func=AF.Sqrt), reads=[kvv], writes=[kvv])
        S.op("dve", lambda e: e.reciprocal(out=rs, in_=vv), reads=[kvv], writes=[krs])
        return rs, krs

    def load_x(layer, s, from_input=False):
        key, ap, sem = xs_ring.next()
        src = xin_v[:, s, :] if from_input else xres_v[:, s, :]
        rd = [] if from_input else [("xres", s)]
        S.dma("sp", ap, src, "ld_" + sem, reads=rd, writes=[key])
        return key, ap

    def store_x(ap, key, s):
        S.dma("sp", xres_v[:, s, :], ap, "st_" + key[0] + str(key[1]), reads=[key], writes=[("xres", s)])

    def compute_mod(l, half, wst_ring, norm_row):
        S.dma("sp", gbc, d_ng[norm_row:norm_row + 1, :].partition_broadcast(128), "gbc", writes=["gbc"])
        for nb in range(6):
            col0 = half * 3072 + nb * 512
            wk, wap, wsem = wst_ring.next()
            S.dma("pool", wap, d_w_ada[l, :, col0:col0 + 512].rearrange("(k p) n -> p k n", p=128), "w_" + wsem,
                  writes=[wk])
            bk, bap, bsem = bb_ring.next()
            S.dma("sp", bap, d_b_ada[l:l + 1, col0:col0 + 512].partition_broadcast(128), "b_" + bsem, writes=[bk])
            bank = rot_z.next()

            def mm(e, wap=wap, bank=bank):
                for k in range(8):
                    ins = e.matmul(ps[bank][:], lhsT=cactT[:, k, :], rhs=wap[:, k, :], start=(k == 0), stop=(k == 7))
                return ins
            S.op("pe", mm, reads=[wk, "cactT"], writes=[PSK(bank)])
            j, h2 = nb // 2, nb % 2
            dst = mod3[:, j, h2 * 512:(h2 + 1) * 512]
            S.op("dve", lambda e, dst=dst, bank=bank, bap=bap: e.tensor_tensor(out=dst, in0=ps[bank][:], in1=bap, op=ALU.add),
                 reads=[PSK(bank), bk], writes=[("mod3", j, h2)])
        S.op("dve", lambda e: e.scalar_tensor_tensor(out=mod3[:, 1, :], in0=mod3[:, 1, :], scalar=1.0, in1=gbc,
                                                     op0=ALU.add, op1=ALU.mult),
             reads=[("mod3", 1, 0), ("mod3", 1, 1), "gbc"], writes=[("mod3", 1, 0), ("mod3", 1, 1)])

    MOD_ALL = [("mod3", j, h) for j in range(3) for h in range(2)]

    def norm_front(xs_ap, xs_key):
        hk, hap, _ = htm_ring.next()
        rs, krs = rmsnorm_rstd(xs_ap, xs_key, hap, hk)
        return (xs_ap, xs_key, hk, hap, rs, krs)

    def norm_back(fr):
        xs_ap, xs_key, hk, hap, rs, krs = fr
        S.op("dve", lambda e: e.scalar_tensor_tensor(out=hap, in0=xs_ap, scalar=rs, in1=mod3[:, 1, :], op0=ALU.mult, op1=ALU.mult),
             reads=[xs_key, krs, ("mod3", 1, 0), ("mod3", 1, 1)], writes=[hk])
        S.op("dve", lambda e: e.tensor_tensor(out=hap, in0=hap, in1=mod3[:, 0, :], op=ALU.add),
             reads=[hk, ("mod3", 0, 0), ("mod3", 0, 1)], writes=[hk])
        return hk, hap

    def transpose_to_fm(hk, hap, dst_fn, dst_keys, extra32=None):
        for half in range(2):
            bank = rot_tr.next()

            def tr(e, half=half, bank=bank):
                for q in range(4):
                    k = half * 4 + q
                    ins = e.transpose(ps[bank][:, q * 128:(q + 1) * 128], hap[:, k * 128:(k + 1) * 128], ident)
                return ins
            S.op("pe", tr, reads=[hk, "ident"], writes=[PSK(bank)])
            dst = dst_fn(half * 4)
            src = ps[bank][:].rearrange("p (a b) -> p a b", b=128)
            if extra32 is not None:
                d32, k32 = extra32(half * 4)
                S.op("act", lambda e, d32=d32, src=src: e.activation(out=d32, in_=src, func=AF.Copy),
                     reads=[PSK(bank)], writes=[k32])
                S.op("dve", lambda e, dst=dst, d32=d32: e.tensor_copy(out=dst, in_=d32),
                     reads=[k32], writes=[dst_keys[half]])
                continue
            eng = "act" if half == 0 else "dve"
            if eng == "act":
                S.op("act", lambda e, dst=dst, src=src: e.activation(out=dst, in_=src, func=AF.Copy),
                     reads=[PSK(bank)], writes=[dst_keys[half]])
            else:
                S.op("dve", lambda e, dst=dst, src=src: e.tensor_copy(out=dst, in_=src),
                     reads=[PSK(bank)], writes=[dst_keys[half]])

    def mixer_phase(l):
        mc = Carver(PH0, ARENA)
        wst = [mc.get(8 * 512 * 2, BF16, (8, 512)) for _ in range(3)]
        wst_ring = Ring("wst", wst)
        wrga = mc.get(8 * 128 * 2, BF16, (8, 128))
        wrgx = mc.get(8 * 128 * 2, BF16, (8, 128))
        wpool = mc.get(4 * 128 * 2, BF16, (4, 128))
        wupa = mc.get(8 * 1024 * 2, BF16, (8, 1024))
        wupb = mc.get(4 * 1024 * 2, BF16, (4, 1024))
        wout = mc.get(8 * 1024 * 2, BF16, (8, 1024))
        dg = mc.get(32 * 128 * 2, BF16, (32, 128))
        hfm = [mc.get(8 * 512 * 2, BF16, (8, 512)) for _ in range(2)]
        ylru = mc.get(8 * 512 * 2, BF16, (8, 512))
        ypool = mc.get(4 * 512 * 2, BF16, (4, 512))
        merged = mc.get(8 * 512 * 2, BF16, (8, 512))
        tmps = [mc.get(512 * 4, F32) for _ in range(16)]
        tmp_ring = Ring("tmp", tmps)
        xlb = [mc.get(516 * 2, BF16) for _ in range(2)]
        xlb_ring = Ring("xlb", xlb)
        xcb = [mc.get(512 * 2, BF16) for _ in range(4)]
        xcb_ring = Ring("xcb", xcb)
        xpb = [mc.get(528 * 4, F32) for _ in range(2)]
        xpb_ring = Ring("xpb", xpb)
        xpt = [mc.get(528 * 4, F32) for _ in range(2)]
        xpt_ring = Ring("xpt", xpt)
        plb = [mc.get(512 * 2, BF16) for _ in range(2)]
        plb_ring = Ring("plb", plb)

        gk = ("mw", l)
        S.dma("pool", wrga, d_w_rg_a[l].rearrange("h i o -> i h o"), "mw", writes=["wrga"], group=gk)
        S.dma("pool", wrgx, d_w_rg_x[l].rearrange("h i o -> i h o"), "mw", writes=["wrgx"], group=gk)
        S.dma("pool", wpool, d_w_pool[l].rearrange("h i o -> i h o"), "mw", writes=["wpool"], group=gk)
        S.dma("pool", wupa, d_w_up_a[l].rearrange("(k p) n -> p k n", p=128), "mw", writes=["wupa"], group=gk)
        S.dma("pool", wupb, d_w_up_b[l].rearrange("(k p) n -> p k n", p=128), "mw", writes=["wupb"], group=gk)
        S.dma("pool", wout, d_w_out[l].rearrange("(k p) n -> p k n", p=128), "mw", writes=["wout"], group=gk)

        compute_mod(l, 0, wst_ring, l)

        for c in range(8):
            for k in range(4):
                j = c * 4 + k
                col = cv(l, "cw", k * 8 + c)
                S.op("dve", lambda e, j=j, col=col: e.tensor_scalar(out=dg[:, j, :], in0=ident, scalar1=cvec[:, col:col + 1],
                                                                   scalar2=None, op0=ALU.mult),
                     reads=["ident", "cvec"], writes=[("dg", j)])
        lam = cvec[:, cv(l, "lam"):cv(l, "lam") + 8]
        t8 = stat[:, 60:68] if False else small[:, 104:112]
        S.op("act", lambda e: e.activation(out=t8, in_=lam, func=AF.Exp, scale=-1.0), reads=["cvec"], writes=["t8"])
        S.op("act", lambda e: e.activation(out=t8, in_=t8, func=AF.Ln, bias=1.0), reads=["t8"], writes=["t8"])
        S.op("dve", lambda e: e.tensor_scalar(out=m8sp, in0=t8, scalar1=-8.0, scalar2=None, op0=ALU.mult), reads=["t8"], writes=["m8sp"])
        S.op("dve", lambda e: e.memset(hcar, 0.0), writes=[("hcar", c) for c in range(8)])
        S.op("dve", lambda e: e.memset(ccar, 0.0), writes=[("ccar", c) for c in range(8)])
        S.op("dve", lambda e: e.memset(pcar, 0.0), writes=[("pcar", g) for g in range(4)])

        n_tiles = S_LEN // 512
        for ti in range(n_tiles):
            hf = hfm[ti % 2]
            hfk = ("hfm", ti % 2)
            def m_front(j, ti=ti):
                xk, xap = load_x(l, ti * 4 + j, from_input=(l == 0))
                return norm_front(xap, xk)
            fr = m_front(0)
            for j in range(4):
                fr_next = m_front(j + 1) if j + 1 < 4 else None
                hk, hap = norm_back(fr)
                fr = fr_next
                transpose_to_fm(hk, hap, lambda k0, j=j, hf=hf: hf[:, k0:k0 + 4, j * 128:(j + 1) * 128],
                                [hfk + (j, 0), hfk + (j, 1)])
            hf_keys = [hfk + (j, h) for j in range(4) for h in range(2)]

            def inproj(wap, wk, q, hf=hf, hf_keys=hf_keys):
                bank = rot_z.next()

                def mm(e, wap=wap, q=q, bank=bank):
                    for k in range(8):
                        ins = e.matmul(ps[bank][:], lhsT=wap[:, k, q * 128:(q + 1) * 128], rhs=hf[:, k, :],
                                       start=(k == 0), stop=(k == 7))
                    return ins
                S.op("pe", mm, reads=[wk] + hf_keys, writes=[PSK(bank)])
                return bank

            def load_piece(col0, ncols=512, part=None, ring_slot=None):
                if ring_slot is None:
                    wk, wap, wsem = wst_ring.next()
                else:
                    wk, wap, wsem = ring_slot
                if part is None:
                    S.dma("pool", wap, d_w_in[l, :, col0:col0 + ncols].rearrange("(k p) n -> p k n", p=128), "w_" + wsem,
                          writes=[wk])
                return wk, wap, wsem

            gq = []
            for pc_ in range(2):
                wk, wap, _ = load_piece(1024 + pc_ * 512)
                for q in range(4):
                    gq.append((pc_ * 4 + q, wap, wk, q))

            def g_stage1(c, wap, wk, q):
                bank = inproj(wap, wk, q)
                gk_, g_, _ = tmp_ring.next()
                tk_, t_, _ = tmp_ring.next()
                S.op("act", lambda e, g_=g_, bank=bank: e.activation(out=g_, in_=ps[bank][:], func=AF.Copy),
                     reads=[PSK(bank)], writes=[gk_])
                S.op("dve", lambda e, g_=g_, t_=t_: e.scalar_tensor_tensor(out=t_, in0=g_, scalar=0.044715, in1=g_,
                                                                          op0=ALU.mult, op1=ALU.mult),
                     reads=[gk_], writes=[tk_])
                S.op("dve", lambda e, g_=g_, t_=t_: e.scalar_tensor_tensor(out=t_, in0=t_, scalar=1.0, in1=g_,
                                                                          op0=ALU.add, op1=ALU.mult),
                     reads=[gk_, tk_], writes=[tk_])
                return (c, gk_, g_, tk_, t_)

            def g_stage2(c, gk_, g_, tk_, t_):
                S.op("act", lambda e, t_=t_: e.activation(out=t_, in_=t_, func=AF.Sigmoid, scale=1.5957691216057308),
                     reads=[tk_], writes=[tk_])
                S.op("dve", lambda e, g_=g_, t_=t_, c=c: e.tensor_tensor(out=ylru[:, c, :], in0=g_, in1=t_, op=ALU.mult),
                     reads=[gk_, tk_], writes=[("ylru", c)])

            pend = g_stage1(*gq[0])
            for n in range(8):
                nxt = g_stage1(*gq[n + 1]) if n + 1 < 8 else None
                g_stage2(*pend)
                pend = nxt

            def lru_front(pc_):
                wk, wap, _ = load_piece(pc_ * 512)
                st8 = [dict() for _ in range(4)]

                def l_stage1(q, wap=wap, wk=wk, pc_=pc_, st8=st8):
                    c = pc_ * 4 + q
                    bank = inproj(wap, wk, q)
                    xk_, xl_, _ = xlb_ring.next()
                    S.op("act", lambda e, xl_=xl_, c=c: e.activation(out=xl_[:, 0:3], in_=ccar[:, c, 0:3], func=AF.Copy),
                         reads=[("ccar", c)], writes=[xk_])
                    S.op("act", lambda e, xl_=xl_, bank=bank: e.activation(out=xl_[:, 3:515], in_=ps[bank][:], func=AF.Copy),
                         reads=[PSK(bank)], writes=[xk_])
                    S.op("act", lambda e, xl_=xl_, c=c: e.activation(out=ccar[:, c, 0:3], in_=xl_[:, 512:515], func=AF.Copy),
                         reads=[xk_], writes=[("ccar", c)])
                    st8[q].update(c=c, xk=xk_, xl=xl_)

                def l_stage2(q, st8=st8):
                    d_ = st8[q]
                    c, xk_, xl_ = d_["c"], d_["xk"], d_["xl"]
                    b2 = rot_aux.next()

                    def conv(e, xl_=xl_, c=c, b2=b2):
                        for k in range(4):
                            ins = e.matmul(ps[b2][:], lhsT=dg[:, c * 4 + k, :], rhs=xl_[:, k:k + 512], start=(k == 0), stop=(k == 3))
                        return ins
                    S.op("pe", conv, reads=[xk_] + [("dg", c * 4 + k) for k in range(4)], writes=[PSK(b2)])
                    xck, xc_, _ = xcb_ring.next()
                    cb_col = cv(l, "cb", c)
                    S.op("act", lambda e, xc_=xc_, b2=b2, cb_col=cb_col: e.activation(out=xc_, in_=ps[b2][:], func=AF.Identity,
                                                                                     bias=cvec[:, cb_col:cb_col + 1]),
                         reads=[PSK(b2), "cvec"], writes=[xck])
                    d_.update(xck=xck, xc=xc_)

                def l_stage3(q, st8=st8):
                    d_ = st8[q]
                    c, xck, xc_ = d_["c"], d_["xck"], d_["xc"]
                    b3 = rot_aux.next()
                    S.op("pe", lambda e, b3=b3, c=c, xc_=xc_: e.matmul(ps[b3][:], lhsT=wrga[:, c, :], rhs=xc_, start=True, stop=True),
                         reads=[xck, "wrga"], writes=[PSK(b3)])
                    b4 = rot_aux.next()
                    S.op("pe", lambda e, b4=b4, c=c, xc_=xc_: e.matmul(ps[b4][:], lhsT=wrgx[:, c, :], rhs=xc_, start=True, stop=True),
                         reads=[xck, "wrgx"], writes=[PSK(b4)])
                    rk, r_, _ = tmp_ring.next()
                    ik, i_, _ = tmp_ring.next()
                    mk, m_, _ = tmp_ring.next()
                    ba_col = cv(l, "ba", c)
                    bx_col = cv(l, "bx", c)
                    S.op("act", lambda e, r_=r_, b3=b3, ba_col=ba_col: e.activation(out=r_, in_=ps[b3][:], func=AF.Sigmoid,
                                                                                   bias=cvec[:, ba_col:ba_col + 1]),
                         reads=[PSK(b3), "cvec"], writes=[rk])
                    S.op("act", lambda e, i_=i_, b4=b4, bx_col=bx_col: e.activation(out=i_, in_=ps[b4][:], func=AF.Sigmoid,
                                                                                   bias=cvec[:, bx_col:bx_col + 1]),
                         reads=[PSK(b4), "cvec"], writes=[ik])
                    d_.update(rk=rk, r=r_, ik=ik, i=i_, mk=mk, m=m_)

                for step in range(6):
                    if step < 4:
                        l_stage1(step)
                    if 1 <= step <= 4:
                        l_stage2(step - 1)
                    if 2 <= step <= 5:
                        l_stage3(step - 2)
                return st8

            def lru_back(st8):
                for q in range(4):
                    d_ = st8[q]
                    S.op("act", lambda e, r_=d_["r"], c=d_["c"]: e.activation(out=r_, in_=r_, func=AF.Exp, scale=m8sp[:, c:c + 1]),
                         reads=[d_["rk"], "m8sp"], writes=[d_["rk"]])
                for q in range(4):
                    d_ = st8[q]
                    S.op("dve", lambda e, a_=d_["r"], m_=d_["m"]: e.scalar_tensor_tensor(out=m_, in0=a_, scalar=-1.0, in1=a_,
                                                                                      op0=ALU.mult, op1=ALU.mult),
                         reads=[d_["rk"]], writes=[d_["mk"]])
                    S.op("dve", lambda e, i_=d_["i"], xc_=d_["xc"]: e.tensor_tensor(out=i_, in0=i_, in1=xc_, op=ALU.mult),
                         reads=[d_["ik"], d_["xck"]], writes=[d_["ik"]])
                for q in range(4):
                    d_ = st8[q]
                    S.op("act", lambda e, m_=d_["m"]: e.activation(out=m_, in_=m_, func=AF.Sqrt, bias=1.0, scale=1.0),
                         reads=[d_["mk"]], writes=[d_["mk"]])
                for q in range(4):
                    d_ = st8[q]
                    c = d_["c"]
                    r_, i_, m_ = d_["r"], d_["i"], d_["m"]
                    rk, ik, mk = d_["rk"], d_["ik"], d_["mk"]
                    S.op("dve", lambda e, i_=i_, m_=m_: e.tensor_tensor(out=i_, in0=i_, in1=m_, op=ALU.mult),
                         reads=[ik, mk], writes=[ik])
                    S.op("dve", lambda e, r_=r_, m_=m_, i_=i_, c=c: e.tensor_tensor_scan(out=m_, data0=r_, data1=i_,
                                                                                        initial=hcar[:, c:c + 1],
                                                                                        op0=ALU.mult, op1=ALU.add),
                         reads=[rk, ik, ("hcar", c), mk], writes=[mk])
                    S.op("act", lambda e, m_=m_, c=c: e.activation(out=hcar[:, c:c + 1], in_=m_[:, 511:512], func=AF.Copy),
                         reads=[mk], writes=[("hcar", c)])
                    S.op("dve", lambda e, m_=m_, c=c: e.tensor_tensor(out=ylru[:, c, :], in0=ylru[:, c, :], in1=m_, op=ALU.mult),
                         reads=[mk, ("ylru", c)], writes=[("ylru", c)])


            def pool_phase():
                wk, wap, _ = load_piece(2048)
                for g in range(4):
                    w = 2 << g
                    bank = inproj(wap, wk, g)
                    pk, pb, _ = xpb_ring.next()
                    S.op("act", lambda e, pb=pb, g=g: e.activation(out=pb[:, 0:15], in_=pcar[:, g, 0:15], func=AF.Copy),
                         reads=[("pcar", g)], writes=[pk])
                    S.op("act", lambda e, pb=pb, bank=bank: e.activation(out=pb[:, 15:527], in_=ps[bank][:], func=AF.Copy),
                         reads=[PSK(bank)], writes=[pk])
                    S.op("act", lambda e, pb=pb, g=g: e.activation(out=pcar[:, g, 0:15], in_=pb[:, 512:527], func=AF.Copy),
                         reads=[pk], writes=[("pcar", g)])
                    cur, curk, lo = pb, pk, 0
                    sh = 1
                    for step in range(g + 1):
                        nk, nb_, _ = xpt_ring.next()
                        n = 527 - (lo + sh)
                        S.op("dve", lambda e, nb_=nb_, cur=cur, lo=lo, sh=sh, n=n: e.tensor_tensor(
                            out=nb_[:, lo + sh:527], in0=cur[:, lo + sh:527], in1=cur[:, lo:lo + n], op=ALU.add),
                            reads=[curk], writes=[nk])
                        cur, curk, lo = nb_, nk, lo + sh
                        sh *= 2
                    plk, pl_, _ = plb_ring.next()
                    S.op("dve", lambda e, pl_=pl_, cur=cur, pb=pb, w=w: e.scalar_tensor_tensor(
                        out=pl_, in0=cur[:, 15:527], scalar=1.0 / w, in1=pb[:, 15:527], op0=ALU.mult, op1=ALU.subtract),
                        reads=[curk, pk], writes=[plk])
                    if ti == 0:
                        tk_, t_, _ = tmp_ring.next()
                        S.op("dve", lambda e, t_=t_, cur=cur, g=g: e.tensor_tensor(out=t_[:, 0:16], in0=cur[:, 15:31], in1=cinv[:, g, :], op=ALU.mult),
                             reads=[curk, ("cinv", g)], writes=[tk_])
                        S.op("dve", lambda e, t_=t_, pl_=pl_, pb=pb: e.tensor_tensor(out=pl_[:, 0:16], in0=t_[:, 0:16], in1=pb[:, 15:31], op=ALU.subtract),
                             reads=[tk_, pk, plk], writes=[plk])
                    b2 = rot_aux.next()
                    S.op("pe", lambda e, b2=b2, g=g, pl_=pl_: e.matmul(ps[b2][:], lhsT=wpool[:, g, :], rhs=pl_, start=True, stop=True),
                         reads=[plk, "wpool"], writes=[PSK(b2)])
                    psc = cv(l, "ps", g)
                    S.op("act", lambda e, g=g, b2=b2, psc=psc: e.activation(out=ypool[:, g, :], in_=ps[b2][:], func=AF.Copy,
                                                                             scale=cvec[:, psc:psc + 1]),
                         reads=[PSK(b2), "cvec"], writes=[("ypool", g)])


            def gates_part(qq):
                wk, wap, wsem = wst_ring.next()
                cA = 2560 + qq * 256
                cB = 2560 + 1024 + qq * 256
                S.dma("pool", wap[:, :, 0:256], d_w_in[l, :, cA:cA + 256].rearrange("(k p) n -> p k n", p=128), "w_" + wsem,
                      writes=[wk])
                S.dma("pool", wap[:, :, 256:512], d_w_in[l, :, cB:cB + 256].rearrange("(k p) n -> p k n", p=128), "w_" + wsem,
                      writes=[wk])
                gts = []
                for q in range(4):
                    bank = rot_z.next()

                    def mm(e, wap=wap, q=q, bank=bank, hf=hf):
                        for k in range(8):
                            ins = e.matmul(ps[bank][:], lhsT=wap[:, k, q * 128:(q + 1) * 128], rhs=hf[:, k, :],
                                           start=(k == 0), stop=(k == 7))
                        return ins
                    S.op("pe", mm, reads=[wk] + hf_keys, writes=[PSK(bank)])
                    gk_, g_, _ = tmp_ring.next()
                    S.op("act", lambda e, g_=g_, bank=bank: e.activation(out=g_, in_=ps[bank][:], func=AF.Sigmoid),
                         reads=[PSK(bank)], writes=[gk_])
                    gts.append((gk_, g_))
                return gts

            def upmerge_part(qq, gts):
                for cc in range(2):
                    c = qq * 2 + cc
                    g0k, g0 = gts[cc]
                    g1k, g1 = gts[2 + cc]
                    ba_ = rot_aux.next()

                    def mma(e, c=c, ba_=ba_):
                        for k in range(8):
                            ins = e.matmul(ps[ba_][:], lhsT=wupa[:, k, c * 128:(c + 1) * 128], rhs=ylru[:, k, :],
                                           start=(k == 0), stop=(k == 7))
                        return ins
                    S.op("pe", mma, reads=["wupa"] + [("ylru", k) for k in range(8)], writes=[PSK(ba_)])
                    bb_ = rot_aux.next()

                    def mmb(e, c=c, bb_=bb_):
                        for k in range(4):
                            ins = e.matmul(ps[bb_][:], lhsT=wupb[:, k, c * 128:(c + 1) * 128], rhs=ypool[:, k, :],
                                           start=(k == 0), stop=(k == 3))
                        return ins
                    S.op("pe", mmb, reads=["wupb"] + [("ypool", k) for k in range(4)], writes=[PSK(bb_)])
                    S.op("dve", lambda e, g0=g0, ba_=ba_: e.tensor_tensor(out=g0, in0=g0, in1=ps[ba_][:], op=ALU.mult),
                         reads=[g0k, PSK(ba_)], writes=[g0k])
                    S.op("dve", lambda e, g1=g1, bb_=bb_: e.tensor_tensor(out=g1, in0=g1, in1=ps[bb_][:], op=ALU.mult),
                         reads=[g1k, PSK(bb_)], writes=[g1k])
                    S.op("dve", lambda e, g0=g0, g1=g1, c=c: e.tensor_tensor(out=merged[:, c, :], in0=g0, in1=g1, op=ALU.add),
                         reads=[g0k, g1k], writes=[("merged", c)])


            st_a = lru_front(0)
            pool_phase()
            lru_back(st_a)
            st_b = lru_front(1)
            gts_cur = gates_part(0)
            lru_back(st_b)
            for qq in range(4):
                gts_next = gates_part(qq + 1) if qq + 1 < 4 else None
                upmerge_part(qq, gts_cur)
                gts_cur = gts_next

            for j in range(4):
                s = ti * 4 + j
                xk, xap = load_x(l, s, from_input=(l == 0))
                for hh in range(2):
                    bank = rot_z.next()

                    def mmo(e, j=j, hh=hh, bank=bank):
                        for k in range(8):
                            ins = e.matmul(ps[bank][:], lhsT=merged[:, k, j * 128:(j + 1) * 128],
                                           rhs=wout[:, k, hh * 512:(hh + 1) * 512], start=(k == 0), stop=(k == 7))
                        return ins
                    S.op("pe", mmo, reads=["wout"] + [("merged", k) for k in range(8)], writes=[PSK(bank)])
                    tk_, t_, _ = tmp_ring.next()
                    S.op("dve", lambda e, t_=t_, bank=bank, hh=hh: e.tensor_tensor(out=t_, in0=ps[bank][:],
                                                                                  in1=mod3[:, 2, hh * 512:(hh + 1) * 512], op=ALU.mult),
                         reads=[PSK(bank), ("mod3", 2, hh)], writes=[tk_])
                    S.op("dve", lambda e, t_=t_, xap=xap, hh=hh: e.tensor_tensor(out=xap[:, hh * 512:(hh + 1) * 512],
                                                                                in0=xap[:, hh * 512:(hh + 1) * 512], in1=t_, op=ALU.add),
                         reads=[tk_, xk], writes=[xk])
                store_x(xap, xk, s)

    def moe_phase(l, last):
        mc = Carver(PH0, ARENA)
        wex = [mc.get(8 * 1024 * 2, BF16, (8, 1024)) for _ in range(4)]
        wex_ring = Ring("wex", wex)
        wst_ring = Ring("wex", [w_[:, :, 0:512] for w_ in wex])
        hfm2 = [mc.get(8 * 1024 * 2, BF16, (8, 1024)) for _ in range(2)]
        yacc = mc.get(8 * 1024 * 4, F32, (8, 1024))
        actb = [mc.get(8 * 512 * 2, BF16, (8, 512)) for _ in range(2)]
        hT32 = [mc.get(8 * 128 * 4, F32, (8, 128)) for _ in range(1)]
        gates2 = [mc.get(8 * 32 * 4, F32, (8, 32)) for _ in range(2)]
        lgb = [mc.get(32 * 4, F32) for _ in range(2)]
        lg_ring = Ring("lg", lgb)
        emb = [mc.get(32 * 4, F32) for _ in range(2)]
        em_ring = Ring("em", emb)
        mx8 = [mc.get(8 * 4, F32) for _ in range(2)]
        mx_ring = Ring("mx", mx8)
        gpad = mc.get(128 * 4, F32)
        gT = [mc.get(128 * 4, F32) for _ in range(2)]
        gT_ring = Ring("gT", gT)
        beo = gbc
        tb0 = mc.off
        tmps = [mc.get(512 * 4, F32) for _ in range(6)]
        tmp_ring = Ring("tmp", tmps)
        tbig = arena[:, tb0:tb0 + 4096].bitcast(F32)
        TBK = [("tmp", 0), ("tmp", 1)]

        compute_mod(l, 1, wst_ring, 4 + l)
        S.op("dve", lambda e: e.memset(gpad, 0.0), writes=["gpad"])
        gk = ("moew", l)
        S.dma("sp", wr32, d_w_router[l].rearrange("(k p) n -> p k n", p=128), "mwc", writes=["wr32"], group=gk)
        S.dma("sp", brbc, d_b_router[l:l + 1, :].partition_broadcast(128), "mwc", writes=["brbc"], group=gk)
        S.dma("sp", b1, d_b1[:, l * 512:(l + 1) * 512].rearrange("p (a b) -> p a b", b=16), "mwc", writes=["b1"], group=gk)
        S.dma("sp", beo[0:32, :], d_b_e_out[l], "gbc", writes=["gbc"])
        S.op("dve", lambda e: e.tensor_scalar(out=b1[:, :, 8:16], in0=b1[:, :, 8:16], scalar1=1.0, scalar2=None, op0=ALU.add),
             reads=["b1"], writes=["b1"])

        rot_gu = Rot([2, 3, 4])
        rot_o = Rot([5, 6])
        MISC = 7

        n_pass = dbg_np
        for p in range(n_pass):
            hfm = hfm2[p % 2]
            gates = gates2[p % 2]

            def e_front(j, q):
                xk, xap = load_x(l, q * 8 + j, from_input=dbg_skip_mixer)
                return norm_front(xap, xk)

            def stage_t(j, q, hk, hap):
                h32 = hT32[0]
                hf_ = hfm2[q % 2]
                transpose_to_fm(hk, hap, lambda k0, j=j, hf_=hf_: hf_[:, k0:k0 + 4, j * 128:(j + 1) * 128],
                                [("hfm", q % 2, j, 0), ("hfm", q % 2, j, 1)],
                                extra32=lambda k0, h32=h32: (h32[:, k0:k0 + 4, :], ("hT32", 0, k0 // 4)))

            def stage_y(j, q, mb):
                h32 = hT32[0]
                gates_ = gates2[q % 2]

                def rmm(e, h32=h32, mb=mb):
                    for k in range(8):
                        ins = e.matmul(ps[mb][:, 0:32], lhsT=h32[:, k, :], rhs=wr32[:, k, :], start=(k == 0), stop=(k == 7))
                    return ins
                S.op("pe", rmm, reads=[("hT32", 0, 0), ("hT32", 0, 1), "wr32"], writes=[PSK(mb)])
                lk, lg, _ = lg_ring.next()
                ek, em, _ = em_ring.next()
                mk_, mx, _ = mx_ring.next()
                S.op("dve", lambda e, lg=lg, mb=mb: e.tensor_tensor(out=lg, in0=ps[mb][:, 0:32], in1=brbc, op=ALU.add),
                     reads=[PSK(mb), "brbc"], writes=[lk])
                S.op("dve", lambda e, lg=lg, mx=mx: e.max(out=mx, in_=lg), reads=[lk], writes=[mk_])
                c0 = stat_col(3)
                negm = stat[:, c0:c0 + 1]
                den = stat[:, c0 + 1:c0 + 2]
                rden = stat[:, c0 + 2:c0 + 3]
                S.op("dve", lambda e, mx=mx, negm=negm: e.tensor_scalar(out=negm, in0=mx[:, 0:1], scalar1=-1.0, scalar2=None, op0=ALU.mult),
                     reads=[mk_], writes=[("stat", c0)])
                S.op("act", lambda e, em=em, lg=lg, negm=negm: e.activation(out=em, in_=lg, func=AF.Exp, bias=negm, scale=1.0),
                     reads=[lk, ("stat", c0)], writes=[ek])
                S.op("dve", lambda e, lg=lg, mx=mx: e.tensor_scalar(out=lg, in0=lg, scalar1=mx[:, 3:4], scalar2=None, op0=ALU.is_ge),
                     reads=[lk, mk_, ek], writes=[lk])
                S.op("dve", lambda e, em=em, lg=lg, den=den: e.scalar_tensor_tensor(out=em, in0=em, scalar=1.0, in1=lg,
                                                                                   op0=ALU.mult, op1=ALU.mult, accum_out=den),
                     reads=[ek, lk], writes=[ek, ("stat", c0 + 1)])
                S.op("dve", lambda e, den=den, rden=rden: e.reciprocal(out=rden, in_=den), reads=[("stat", c0 + 1)], writes=[("stat", c0 + 2)])
                S.op("dve", lambda e, em=em, rden=rden, j=j, gates_=gates_: e.tensor_scalar(out=gates_[:, j, :], in0=em, scalar1=rden, scalar2=None, op0=ALU.mult),
                     reads=[ek, ("stat", c0 + 2)], writes=[("gates", q % 2, j)])

            def stage_z(j):
                mb = 4
                S.op("dve", lambda e, j=j, gates=gates: e.tensor_copy(out=gpad[:, 0:32], in_=gates[:, j, :]),
                     reads=[("gates", p % 2, j)], writes=["gpad"])
                S.op("pe", lambda e, mb=mb: e.transpose(ps[mb][:, 128:256], gpad, ident),
                     reads=["gpad", "ident"], writes=[PSK(mb)])
                gtk, gt, _ = gT_ring.next()
                S.op("act", lambda e, gt=gt, mb=mb: e.activation(out=gt[0:32, :], in_=ps[mb][0:32, 128:256], func=AF.Copy),
                     reads=[PSK(mb)], writes=[gtk])
                for hh in range(2):
                    bo = rot_o.next()
                    S.op("pe", lambda e, gt=gt, hh=hh, bo=bo: e.matmul(ps[bo][:], lhsT=gt[0:32, :], rhs=beo[0:32, hh * 512:(hh + 1) * 512],
                                                                      start=True, stop=True),
                         reads=[gtk, "gbc"], writes=[PSK(bo)])
                    S.op("act", lambda e, j=j, hh=hh, bo=bo: e.activation(out=yacc[:, j, hh * 512:(hh + 1) * 512], in_=ps[bo][:], func=AF.Copy),
                         reads=[PSK(bo)], writes=[("yacc", j, hh)])

            if p == 0:
                for j in range(8):
                    hk, hap = norm_back(e_front(j, 0))
                    stage_t(j, 0, hk, hap)
                    stage_y(j, 0, 2 + (j % 2))
            for j in range(8):
                stage_z(j)
            sched = {}
            if p + 1 < n_pass:
                hold = {}

                def mk_f(j):
                    def f():
                        hold[j] = norm_back(e_front(j, p + 1))
                    return f

                def mk_t(j):
                    def f():
                        hk, hap = hold.pop(j)
                        stage_t(j, p + 1, hk, hap)
                    return f

                def mk_y(j):
                    def f():
                        stage_y(j, p + 1, 7)
                    return f
                for j in range(8):
                    sched.setdefault(3 + 3 * j, []).append(mk_f(j))
                    sched.setdefault(4 + 3 * j, []).append(mk_t(j))
                    sched.setdefault(5 + 3 * j, []).append(mk_y(j))

            hf_keys = [[("hfm", p % 2, g * 4 + j4, h) for j4 in range(4) for h in range(2)] for g in range(2)]
            for ex in range(dbg_ne):
                for fn_ in sched.get(ex, []):
                    fn_()
                wgk, wg, wgs = wex_ring.next()
                S.dma("pool", wg, d_w_e_in[l, ex, :, 0:1024].rearrange("(k p) n -> p k n", p=128), "e_" + wgs, writes=[wgk])
                wuk, wu, wus = wex_ring.next()
                S.dma("pool", wu, d_w_e_in[l, ex, :, 1024:2048].rearrange("(k p) n -> p k n", p=128), "e_" + wus, writes=[wuk])
                wok, wo, wos = wex_ring.next()
                S.dma("pool", wo, d_w_e_out[l, ex].rearrange("(k p) n -> p k n", p=128), "e_" + wos, writes=[wok])
                pend_tail = [None]
                for g in range(2):
                    ab = actb[g]
                    for jp in range(8):
                        bg = rot_gu.next()

                        def mg(e, wg=wg, jp=jp, bg=bg, g=g, hfm=hfm):
                            for k in range(8):
                                ins = e.matmul(ps[bg][:], lhsT=wg[:, k, jp * 128:(jp + 1) * 128], rhs=hfm[:, k, g * 512:(g + 1) * 512],
                                               start=(k == 0), stop=(k == 7))
                            return ins
                        S.op("pe", mg, reads=[wgk] + hf_keys[g], writes=[PSK(bg)])
                        bu = rot_gu.next()

                        def mu(e, wu=wu, jp=jp, bu=bu, g=g, hfm=hfm):
                            for k in range(8):
                                ins = e.matmul(ps[bu][:], lhsT=wu[:, k, jp * 128:(jp + 1) * 128], rhs=hfm[:, k, g * 512:(g + 1) * 512],
                                               start=(k == 0), stop=(k == 7))
                            return ins
                        S.op("pe", mu, reads=[wuk] + hf_keys[g], writes=[PSK(bu)])
                        gck, gc, _ = tmp_ring.next()
                        sgk, sg, _ = tmp_ring.next()
                        uck, uc, _ = tmp_ring.next()
                        S.op("dve", lambda e, gc=gc, bg=bg, ex=ex, jp=jp: e.tensor_scalar(out=gc, in0=ps[bg][:], scalar1=b1[:, ex, jp:jp + 1],
                                                                                         scalar2=7.0, op0=ALU.add, op1=ALU.min),
                             reads=[PSK(bg), "b1"], writes=[gck])
                        S.op("act", lambda e, sg=sg, gc=gc: e.activation(out=sg, in_=gc, func=AF.Sigmoid, scale=1.702),
                             reads=[gck], writes=[sgk])
                        S.op("dve", lambda e, uc=uc, bu=bu, ex=ex, jp=jp: e.tensor_scalar(out=uc, in0=ps[bu][:], scalar1=b1[:, ex, 8 + jp:9 + jp],
                                                                                         scalar2=8.0, op0=ALU.add, op1=ALU.min),
                             reads=[PSK(bu), "b1"], writes=[uck])
                        def tail(gc=gc, sg=sg, uc=uc, gck=gck, sgk=sgk, uck=uck, ab=ab, jp=jp, g=g):
                            S.op("dve", lambda e: e.tensor_tensor(out=gc, in0=gc, in1=sg, op=ALU.mult),
                                 reads=[gck, sgk], writes=[gck])
                            S.op("dve", lambda e: e.scalar_tensor_tensor(out=ab[:, jp, :], in0=uc, scalar=-6.0, in1=gc,
                                                                         op0=ALU.max, op1=ALU.mult),
                                 reads=[uck, gck], writes=[("act", g, jp)])
                        if pend_tail[0] is not None:
                            pend_tail[0]()
                        pend_tail[0] = tail
                    pend_tail[0]()
                    pend_tail[0] = None
                for g in range(2):
                    ab = actb[g]
                    for j4 in range(4):
                        js = g * 4 + j4
                        for hh in range(2):
                            bo = rot_o.next()

                            def mo(e, ab=ab, j4=j4, hh=hh, bo=bo, wo=wo):
                                for k in range(8):
                                    ins = e.matmul(ps[bo][:], lhsT=ab[:, k, j4 * 128:(j4 + 1) * 128], rhs=wo[:, k, hh * 512:(hh + 1) * 512],
                                                   start=(k == 0), stop=(k == 7))
                                return ins
                            S.op("pe", mo, reads=[wok] + [("act", g, k) for k in range(8)], writes=[PSK(bo)])
                            S.op("dve", lambda e, js=js, hh=hh, bo=bo, ex=ex, gates=gates: e.scalar_tensor_tensor(
                                out=yacc[:, js, hh * 512:(hh + 1) * 512], in0=ps[bo][:], scalar=gates[:, js, ex:ex + 1],
                                in1=yacc[:, js, hh * 512:(hh + 1) * 512], op0=ALU.mult, op1=ALU.add),
                                reads=[PSK(bo), ("gates", p % 2, js), ("yacc", js, hh)], writes=[("yacc", js, hh)])
            for j in range(8 if (dbg_cut > 0 and not (dbg_f & 2)) else 0):
                s = p * 8 + j
                xk, xap = load_x(l, s, from_input=dbg_skip_mixer)
                S.op("dve", lambda e, j=j: e.tensor_tensor(out=tbig, in0=yacc[:, j, :], in1=mod3[:, 2, :], op=ALU.mult),
                     reads=[("yacc", j, 0), ("yacc", j, 1), ("mod3", 2, 0), ("mod3", 2, 1)], writes=TBK)
                S.op("dve", lambda e, xap=xap: e.tensor_tensor(out=xap, in0=xap, in1=tbig, op=ALU.add),
                     reads=TBK + [xk], writes=[xk])
                store_x(xap, xk, s)

    final_ops = []
    for l in range(n_layers):
        if not dbg_skip_mixer:
            mixer_phase(l)
            S.barrier()
        if stop_after == ("mixer", l):
            break
        moe_phase(l, last=(l == n_layers - 1 and stop_after is None))
        S.barrier()
        if stop_after == ("moe", l):
            break
    if stop_after is None:
        S.dma("sp", gbc, d_ng[8:9, :].partition_broadcast(128), "gbc", writes=["gbc"])
        for s in range(S_LEN // 128):
            xk, xap = load_x(1, s)
            hk, hap, _ = htm_ring.next()
            rs, krs = rmsnorm_rstd(xap, xk, hap, hk)
            S.op("dve", lambda e, xap=xap, rs=rs: e.scalar_tensor_tensor(out=xap, in0=xap, scalar=rs, in1=gbc, op0=ALU.mult, op1=ALU.mult),
                 reads=[xk, krs, "gbc"], writes=[xk])
            o = S.dma("sp", out_v[:, s, :], xap, "st_" + xk[0] + str(xk[1]), reads=[xk], writes=[("out", s)])
            final_ops.append(o)
    if not final_ops:
        for s in range(32):
            xk, xap = load_x(1, s)
            o = S.dma("sp", out_v[:, s, :], xap, "st_" + xk[0] + str(xk[1]), reads=[xk], writes=[("out", s)])
            final_ops.append(o)
    S.emit(final_wait_ops=final_ops)
    st.close()
    return nc


def make_in_maps(inp):
    f = lambda a: np.ascontiguousarray(np.asarray(a, dtype=np.float32))
    L = L_DEPTH
    cvec = np.zeros((128, L * NCV), np.float32)

    def fm(v):
        return np.asarray(v, np.float32).reshape(-1, 128).T

    for l in range(L):
        base = l * NCV
        for k in range(4):
            cvec[:, base + k * 8: base + k * 8 + 8] = fm(inp["conv_w"][l, k])
        cvec[:, base + 32: base + 40] = fm(inp["conv_b"][l])
        cvec[:, base + 40: base + 48] = fm(inp["b_rg_a"][l])
        cvec[:, base + 48: base + 56] = fm(inp["b_rg_x"][l])
        cvec[:, base + 56: base + 64] = fm(inp["lru_lambda"][l])
        cvec[:, base + 64: base + 68] = fm(inp["pool_scale"][l])
    b1 = np.zeros((128, L * 512), np.float32)
    be = np.asarray(inp["b_e_in"], np.float32)
    for l in range(L):
        b1[:, l * 512:(l + 1) * 512] = be[l].reshape(NE, 16, 128).transpose(2, 0, 1).reshape(128, 512)
    ng = np.concatenate([np.asarray(inp["norm1_g"], np.float32), np.asarray(inp["norm2_g"], np.float32),
                         np.asarray(inp["final_g"], np.float32)[None, :]], axis=0)
    ident = np.eye(128, dtype=np.float32)
    t = np.arange(512, dtype=np.float32)
    cinv = np.stack([1.0 / np.minimum(t[:16] + 1.0, float(w)) for w in (2, 4, 8, 16)]).astype(np.float32)
    shared = dict(
        w_ada=f(inp["w_ada"]), b_ada=f(inp["b_ada"]), w_in=f(inp["w_in"]), w_rg_a=f(inp["w_rg_a"]), w_rg_x=f(inp["w_rg_x"]),
        w_pool=f(inp["w_pool"]), w_up_a=f(inp["w_up_a"]), w_up_b=f(inp["w_up_b"]), w_out=f(inp["w_out"]),
        w_router=f(inp["w_router"]), b_router=f(inp["b_router"]), w_e_in=f(inp["w_e_in"]), b1=b1,
        w_e_out=f(inp["w_e_out"]), b_e_out=f(inp["b_e_out"]), ng=f(ng), cvec=cvec, ident=ident, cinv=cinv)
    x = f(inp["x"])
    c = np.asarray(inp["c"], np.float32)
    maps = []
    for b in range(x.shape[0]):
        m = dict(shared)
        m["x"] = x[b]
        m["cfm"] = np.ascontiguousarray(fm(c[b]))
        maps.append(m)
    return maps


_CACHE = {}


def kernel(**inputs):
    maps = make_in_maps(inputs)
    if "nc" not in _CACHE:
        _CACHE["nc"] = build_program()
    nc = _CACHE["nc"]
    res = run_bass_kernel_spmd(nc, maps, core_ids=list(range(len(maps))))
    out = np.stack([np.asarray(r["out"], dtype=np.float32) for r in res.results], axis=0)
    return out
```
